# Optimizing a Trainium2 kernel written in Bass

```python
import math
import jax
import jax.numpy as jnp
from jax import lax
import numpy as np

D_MODEL = 2048
BATCH = 8
SEQ = 2048
DEPTH = 4

GRID_W = 64
MIX_W = D_MODEL * 3 // 8
NA_HEAD_DIM = 64
NA_HEADS = MIX_W // NA_HEAD_DIM
NA_W = NA_HEADS * NA_HEAD_DIM
NA_KH_MAX = 8
NA_KW = 16
NA_QB = 16
NA_KB = 32
HY_W = MIX_W
HY_ORDER = 2
HY_SHORT_K = 3
HY_POS_DIM = 33
HY_FILT_HID = 64
HY_FAST_DECAY = 0.3
HY_SLOW_DECAY = 1.5
HY_DECAY_TARGET = 1e-2
CF_W = MIX_W
CF_K = 31
N_GROUPS = 4
EXPERTS_PER_GROUP = 4
N_EXPERTS = N_GROUPS * EXPERTS_PER_GROUP
TOP_K = 2
D_EXPERT = D_MODEL // 2
C_IN = 3 * NA_W + 3 * HY_W + 2 * CF_W + 3 * D_MODEL
ALPHA = (2 * DEPTH) ** 0.25
BETA = (8 * DEPTH) ** -0.25
LN_EPS = 1e-5
NEG_INF = -1e30

kernel_name = 'hybrid_natten_hyena_conformer_moe_encoder'


def layer_norm(x, g, b):
    xf = x.astype(jnp.float32)
    mu = jnp.mean(xf, axis=-1, keepdims=True)
    xc = xf - mu
    var = jnp.mean(xc * xc, axis=-1, keepdims=True)
    y = xc * lax.rsqrt(var + LN_EPS) * g.astype(jnp.float32) + b.astype(jnp.float32)
    return y.astype(x.dtype)


def depthwise_conv_same(u, w, b):
    k = w.shape[0]
    pad = k // 2
    out = lax.conv_general_dilated(u, w[:, None, :].astype(u.dtype), window_strides=(1,),
                                   padding=[(pad, pad)],
                                   dimension_numbers=('NWC', 'WIO', 'NWC'),
                                   feature_group_count=u.shape[-1])
    return out + b.astype(u.dtype)


def neighbourhood_attention(q, k, v, rpb):
    B, L, _ = q.shape
    rows = L // GRID_W
    kh = min(NA_KH_MAX, rows)

    def to_grid(t):
        return t.reshape(B, rows, GRID_W, NA_HEADS, NA_HEAD_DIM).transpose(0, 3, 1, 2, 4)

    qg = to_grid(q * (NA_HEAD_DIM ** -0.5))
    kg = to_grid(k)
    vg = to_grid(v)

    n_cb = GRID_W // NA_QB
    qc = np.arange(GRID_W).reshape(n_cb, NA_QB)
    band0 = np.clip(np.arange(n_cb) * NA_QB - NA_KW // 2, 0, GRID_W - NA_KB)
    kc = band0[:, None] + np.arange(NA_KB)
    cs = np.clip(qc - NA_KW // 2, 0, GRID_W - NA_KW)
    col_valid = (kc[:, None, :] >= cs[:, :, None]) & (kc[:, None, :] < cs[:, :, None] + NA_KW)
    col_off = np.clip(kc[:, None, :] - qc[:, :, None], -(NA_KW - 1), NA_KW - 1) + (NA_KW - 1)
    col_valid = jnp.asarray(col_valid)
    col_off = jnp.asarray(col_off.astype(np.int32))
    kc_flat = jnp.asarray(kc.reshape(-1).astype(np.int32))
    rpb32 = rpb.astype(jnp.float32)

    def row_block(r):
        rs = jnp.clip(r - kh // 2, 0, rows - kh)
        k_rows = lax.dynamic_slice_in_dim(kg, rs, kh, axis=2)
        v_rows = lax.dynamic_slice_in_dim(vg, rs, kh, axis=2)
        k_band = jnp.take(k_rows, kc_flat, axis=3).reshape(B, NA_HEADS, kh, n_cb, NA_KB, NA_HEAD_DIM)
        v_band = jnp.take(v_rows, kc_flat, axis=3).reshape(B, NA_HEADS, kh, n_cb, NA_KB, NA_HEAD_DIM)
        q_row = lax.dynamic_index_in_dim(qg, r, axis=2, keepdims=False)
        q_row = q_row.reshape(B, NA_HEADS, n_cb, NA_QB, NA_HEAD_DIM)
        s = jnp.einsum('bhcqd,bhkcjd->bhcqkj', q_row, k_band).astype(jnp.float32)
        row_off = rs + jnp.arange(kh) - r + (NA_KH_MAX - 1)
        bias = jnp.take(rpb32, row_off, axis=1)
        bias = bias[:, :, col_off].transpose(0, 2, 3, 1, 4)
        bias = jnp.where(col_valid[None, :, :, None, :], bias, NEG_INF)
        s = s + bias[None]
        p = jax.nn.softmax(s.reshape(B, NA_HEADS, n_cb, NA_QB, kh * NA_KB), axis=-1)
        p = p.reshape(s.shape).astype(v.dtype)
        o = jnp.einsum('bhcqkj,bhkcjd->bhcqd', p, v_band)
        return o.reshape(B, NA_HEADS, GRID_W, NA_HEAD_DIM)

    out = lax.map(row_block, jnp.arange(rows))
    return out.transpose(1, 0, 3, 2, 4).reshape(B, L, NA_W)


def hyena_filter_spectra(L, w1, b1, w2, b2, w3, b3, freq, w4):
    f32 = jnp.float32
    t = jnp.linspace(0.0, 1.0, L, dtype=f32)[:, None]
    bands = (HY_POS_DIM - 1) // 2
    w = 2.0 * math.pi * jnp.arange(L, dtype=f32)[:, None] / L
    f = jnp.linspace(1e-4, bands - 1, bands, dtype=f32)[None, :]
    z = jnp.concatenate([t, jnp.cos(f * w), -jnp.sin(f * w)], axis=-1)
    fr = freq.astype(f32)
    h = jnp.sin(fr * (z @ w1.astype(f32) + b1.astype(f32)))
    h = jnp.sin(fr * (h @ w2.astype(f32) + b2.astype(f32)))
    h = jnp.sin(fr * (h @ w3.astype(f32) + b3.astype(f32)))
    h = h @ w4.astype(f32)
    max_decay = math.log(HY_DECAY_TARGET) / HY_FAST_DECAY
    min_decay = math.log(HY_DECAY_TARGET) / HY_SLOW_DECAY
    deltas = jnp.abs(jnp.linspace(min_decay, max_decay, h.shape[-1], dtype=f32))
    h = h * jnp.exp(-t * deltas)
    h = h.reshape(L, 2, HY_ORDER, HY_W)
    filt = jnp.concatenate([h[:, 0], jnp.zeros((1, HY_ORDER, HY_W), f32), h[:0:-1, 1]], axis=0)
    return jnp.fft.rfft(filt, axis=0)


def hyena_mixer(u, conv_w, conv_b, w1, b1, w2, b2, w3, b3, freq, w4, skip):
    B, L, _ = u.shape
    uc = depthwise_conv_same(u, conv_w, conv_b).astype(jnp.float32)
    vv, x1, x2 = jnp.split(uc, 3, axis=-1)
    spec = hyena_filter_spectra(L, w1, b1, w2, b2, w3, b3, freq, w4)
    skip32 = skip.astype(jnp.float32)

    def long_conv(z, o):
        zf = jnp.fft.rfft(z, n=2 * L, axis=1)
        y = jnp.fft.irfft(zf * spec[None, :, o, :], n=2 * L, axis=1)[:, :L]
        return y + z * skip32[o]

    z = x1 * long_conv(vv, 0)
    z = x2 * long_conv(z, 1)
    return z.astype(u.dtype)


def conformer_conv(u, dw_w, dw_b, ln_g, ln_b):
    a, g = jnp.split(u, 2, axis=-1)
    z = a * jax.nn.sigmoid(g)
    z = depthwise_conv_same(z, dw_w, dw_b)
    z = layer_norm(z, ln_g, ln_b)
    return jax.nn.silu(z)


def grouped_moe(x, w_router, b_router, w_gate, w_up, w_down):
    B, L, D = x.shape
    xt = x.reshape(B * L, D)
    logits = (xt @ w_router + b_router).astype(jnp.float32)
    probs = jax.nn.softmax(logits, axis=-1).reshape(-1, N_GROUPS, EXPERTS_PER_GROUP)
    group_score = lax.top_k(probs, TOP_K)[0].sum(-1)
    g_sel = jnp.argmax(group_score, axis=-1)
    p_grp = jnp.take_along_axis(probs, g_sel[:, None, None], axis=1)[:, 0]
    top_p, top_i = lax.top_k(p_grp, TOP_K)
    top_w = top_p / jnp.sum(top_p, axis=-1, keepdims=True)
    expert_id = g_sel[:, None] * EXPERTS_PER_GROUP + top_i
    combine = jnp.sum(jax.nn.one_hot(expert_id, N_EXPERTS, dtype=jnp.float32) * top_w[..., None], axis=1)
    combine = combine.astype(x.dtype)
    y = jnp.zeros_like(xt)
    for e in range(N_EXPERTS):
        h = jax.nn.silu(xt @ w_gate[e]) * (xt @ w_up[e])
        y = y + combine[:, e:e + 1] * (h @ w_down[e])
    return y.reshape(B, L, D)


def setup_inputs(seed: int = 0) -> dict:
    key = jax.random.key(seed)
    ks = jax.random.split(key, 35)
    f32 = jnp.float32

    def nrm(i, shape, std):
        return jax.random.normal(ks[i], shape, f32) * std

    col_scale = np.ones((C_IN,), np.float32)
    col_scale[2 * NA_W:3 * NA_W] = BETA
    return {
        'x': nrm(0, (BATCH, SEQ, D_MODEL), 1.0),
        'in_ln_g': 1.0 + nrm(1, (D_MODEL,), 0.01),
        'in_ln_b': nrm(2, (D_MODEL,), 0.01),
        'w_in': nrm(3, (DEPTH, D_MODEL, C_IN), D_MODEL ** -0.5) * jnp.asarray(col_scale),
        'b_in': nrm(4, (DEPTH, C_IN), 0.01),
        'attn_rpb': nrm(5, (DEPTH, NA_HEADS, 2 * NA_KH_MAX - 1, 2 * NA_KW - 1), 0.02),
        'hy_conv_w': nrm(6, (DEPTH, HY_SHORT_K, 3 * HY_W), HY_SHORT_K ** -0.5),
        'hy_conv_b': nrm(7, (DEPTH, 3 * HY_W), 0.01),
        'hy_f_w1': nrm(8, (DEPTH, HY_POS_DIM, HY_FILT_HID), HY_POS_DIM ** -0.5),
        'hy_f_b1': nrm(9, (DEPTH, HY_FILT_HID), 0.1),
        'hy_f_w2': nrm(10, (DEPTH, HY_FILT_HID, HY_FILT_HID), HY_FILT_HID ** -0.5),
        'hy_f_b2': nrm(11, (DEPTH, HY_FILT_HID), 0.1),
        'hy_f_w3': nrm(12, (DEPTH, HY_FILT_HID, HY_FILT_HID), HY_FILT_HID ** -0.5),
        'hy_f_b3': nrm(13, (DEPTH, HY_FILT_HID), 0.1),
        'hy_f_freq': 1.0 + nrm(14, (DEPTH, HY_FILT_HID), 0.01),
        'hy_f_w4': nrm(15, (DEPTH, HY_FILT_HID, 2 * HY_ORDER * HY_W), 0.01),
        'hy_skip': nrm(16, (DEPTH, HY_ORDER, HY_W), 0.5),
        'cf_dw_w': nrm(17, (DEPTH, CF_K, CF_W), CF_K ** -0.5),
        'cf_dw_b': nrm(18, (DEPTH, CF_W), 0.01),
        'cf_ln_g': 1.0 + nrm(19, (DEPTH, CF_W), 0.01),
        'cf_ln_b': nrm(20, (DEPTH, CF_W), 0.01),
        'w_attn_br': nrm(21, (DEPTH, NA_W, D_MODEL), NA_W ** -0.5 * BETA),
        'w_hy_br': nrm(22, (DEPTH, HY_W, D_MODEL), HY_W ** -0.5 * BETA),
        'w_cf_br': nrm(23, (DEPTH, CF_W, D_MODEL), CF_W ** -0.5 * BETA),
        'w_o': nrm(24, (DEPTH, D_MODEL, D_MODEL), D_MODEL ** -0.5 * BETA),
        'b_o': nrm(25, (DEPTH, D_MODEL), 0.01),
        'ln1_g': 1.0 + nrm(26, (DEPTH, D_MODEL), 0.01),
        'ln1_b': nrm(27, (DEPTH, D_MODEL), 0.01),
        'w_router': nrm(28, (D_MODEL, N_EXPERTS), D_MODEL ** -0.5),
        'b_router': nrm(29, (N_EXPERTS,), 0.01),
        'moe_w_gate': nrm(30, (DEPTH, N_EXPERTS, D_MODEL, D_EXPERT), D_MODEL ** -0.5),
        'moe_w_up': nrm(31, (DEPTH, N_EXPERTS, D_MODEL, D_EXPERT), D_MODEL ** -0.5 * BETA),
        'moe_w_down': nrm(32, (DEPTH, N_EXPERTS, D_EXPERT, D_MODEL), D_EXPERT ** -0.5 * BETA),
        'ln2_g': 1.0 + nrm(33, (DEPTH, D_MODEL), 0.01),
        'ln2_b': nrm(34, (DEPTH, D_MODEL), 0.01),
    }


def reference(x, in_ln_g, in_ln_b, w_in, b_in, attn_rpb, hy_conv_w, hy_conv_b,
              hy_f_w1, hy_f_b1, hy_f_w2, hy_f_b2, hy_f_w3, hy_f_b3, hy_f_freq, hy_f_w4,
              hy_skip, cf_dw_w, cf_dw_b, cf_ln_g, cf_ln_b, w_attn_br, w_hy_br, w_cf_br,
              w_o, b_o, ln1_g, ln1_b, w_router, b_router, moe_w_gate, moe_w_up,
              moe_w_down, ln2_g, ln2_b):
    split_pts = [NA_W, 2 * NA_W, 3 * NA_W, 3 * NA_W + 3 * HY_W,
                 3 * NA_W + 3 * HY_W + 2 * CF_W,
                 3 * NA_W + 3 * HY_W + 2 * CF_W + D_MODEL,
                 3 * NA_W + 3 * HY_W + 2 * CF_W + 2 * D_MODEL]
    h = layer_norm(x, in_ln_g, in_ln_b)
    for l in range(DEPTH):
        p = h @ w_in[l] + b_in[l]
        q, k, v, hy_in, cf_in, g_a, g_h, g_c = jnp.split(p, split_pts, axis=-1)
        y_a = neighbourhood_attention(q, k, v, attn_rpb[l])
        y_h = hyena_mixer(hy_in, hy_conv_w[l], hy_conv_b[l], hy_f_w1[l], hy_f_b1[l],
                          hy_f_w2[l], hy_f_b2[l], hy_f_w3[l], hy_f_b3[l], hy_f_freq[l],
                          hy_f_w4[l], hy_skip[l])
        y_c = conformer_conv(cf_in, cf_dw_w[l], cf_dw_b[l], cf_ln_g[l], cf_ln_b[l])
        merged = (jax.nn.sigmoid(g_a) * (y_a @ w_attn_br[l])
                  + jax.nn.sigmoid(g_h) * (y_h @ w_hy_br[l])
                  + jax.nn.sigmoid(g_c) * (y_c @ w_cf_br[l]))
        mix = merged @ w_o[l] + b_o[l]
        h = layer_norm(ALPHA * h + mix, ln1_g[l], ln1_b[l])
        ffn = grouped_moe(h, w_router, b_router, moe_w_gate[l], moe_w_up[l], moe_w_down[l])
        h = layer_norm(ALPHA * h + ffn, ln2_g[l], ln2_b[l])
    return h
```

```python
import math
from contextlib import ExitStack
import numpy as np
import concourse.bass as bass
import concourse.mybir as mybir
from concourse.bass_utils import run_bass_kernel_spmd

F32 = mybir.dt.float32
F32R = mybir.dt.float32r
AF = mybir.ActivationFunctionType
ALU = mybir.AluOpType

D = 2048
T = 2048
DEPTH = 4
GW = 64
MIXW = 768
CIN = 12288
NEXP = 16
DEXP = 1024
ALPHA = (2 * DEPTH) ** 0.25
LN_EPS = 1e-5
O_Q, O_K, O_V, O_HY, O_CF, O_GA, O_GH, O_GC = 0, 768, 1536, 2304, 4608, 6144, 8192, 10240
PI = math.pi


class Buf:
    def __init__(self, name=""):
        self.w = {}
        self.r = {}
        self.sem = None
        self.name = name


class Sched:
    ENGS = ("pe", "act", "dve", "pool", "sp")

    def __init__(self, nc):
        self.nc = nc
        self.eng = {"pe": nc.tensor, "act": nc.scalar, "dve": nc.vector, "pool": nc.gpsimd, "sp": nc.sync}
        self.q = {e: [] for e in self.ENGS}
        self.sems = []
        self.esem = {e: self._newsem("e_" + e) for e in ("pe", "act", "dve", "pool")}
        self.known = {e: {} for e in self.ENGS}
        self.dma_pool = []
        self.dma_next = 0
        self.nins = 0

    def _newsem(self, name):
        self.sems.append([self.nc.alloc_semaphore(name), 0])
        return len(self.sems) - 1

    def _dma_sem(self):
        if self.dma_next == len(self.dma_pool):
            self.dma_pool.append(self._newsem("d%d" % len(self.dma_pool)))
        self.dma_next += 1
        return self.dma_pool[self.dma_next - 1]

    @staticmethod
    def _deps(reads, writes, accw):
        d = {}

        def add(m):
            for k, v in m.items():
                if d.get(k, 0) < v:
                    d[k] = v
        for b in reads:
            add(b.w)
        for b in writes:
            add(b.w)
            add(b.r)
        for b in accw:
            add(b.r)
        return d

    @staticmethod
    def _commit(sid, val, reads, writes, accw):
        for b in reads:
            if b.r.get(sid, 0) < val:
                b.r[sid] = val
        for b in writes:
            b.w = {sid: val}
            b.r = {}
        for b in accw:
            if b.w.get(sid, 0) < val:
                b.w[sid] = val

    def _filter(self, eng, d):
        out = []
        kn = self.known[eng]
        for k, v in d.items():
            if kn.get(k, 0) < v:
                kn[k] = v
                out.append((k, v))
        return out

    def op(self, eng, fn, reads=(), writes=(), accw=()):
        d = self._deps(reads, writes, accw)
        sid = self.esem[eng]
        if eng == "pe":
            d.pop(sid, None)
        waits = self._filter(eng, d)
        self.sems[sid][1] += 1
        self.q[eng].append((waits, fn, sid, 1))
        self._commit(sid, self.sems[sid][1], reads, writes, accw)

    def dma(self, out, in_, sb, reads=(), writes=(), accw=(), q="sp", split=None):
        if sb.sem is None:
            sb.sem = self._dma_sem()
        sid = sb.sem
        d = self._deps(reads, writes, accw)
        waits = self._filter(q, d)
        pairs = [(out, in_)]
        if split is not None and len(out.shape) >= 3 and out.shape[1] > split:
            n1 = out.shape[1]
            pairs = [(out[:, a:min(a + split, n1)], in_[:, a:min(a + split, n1)]) for a in range(0, n1, split)]
        for i, (o, ii) in enumerate(pairs):
            self.sems[sid][1] += 16
            self.q[q].append((waits if i == 0 else [], lambda e, o=o, ii=ii: e.dma_start(out=o, in_=ii), sid, 16))
        self._commit(sid, self.sems[sid][1], reads, writes, accw)

    def load(self, tile_ap, dram_ap, sb, db, q="sp", split=2):
        self.dma(tile_ap, dram_ap, sb, reads=(db,), writes=(sb,), q=q, split=split)

    def store(self, dram_ap, tile_ap, sb, db, q="pool", split=2):
        self.dma(dram_ap, tile_ap, sb, reads=(sb,), accw=(db,), q=q, split=split)

    def barrier(self):
        allv = {i: c for i, (h, c) in enumerate(self.sems) if c > 0}
        for e in self.ENGS:
            waits = self._filter(e, dict(allv))
            if waits:
                self.q[e].append((waits, None, None, 0))

    def emit(self):
        nc = self.nc
        q = self.q
        sems = self.sems
        with nc.Block() as blk:
            def run(e, items):
                for waits, fn, sid, inc in items:
                    for k, v in waits:
                        e.wait_ge(sems[k][0], v)
                    if fn is not None:
                        fn(e).then_inc(sems[sid][0], inc)
                        self.nins += 1

            @blk.tensor
            def _(e):
                run(e, q["pe"])

            @blk.scalar
            def _(e):
                run(e, q["act"])

            @blk.vector
            def _(e):
                run(e, q["dve"])

            @blk.gpsimd
            def _(e):
                run(e, q["pool"])

            @blk.sync
            def _(e):
                run(e, q["sp"])
        self.q = {e: [] for e in self.ENGS}
        self.dma_next = 0


class Phase:
    uid = 0

    def __init__(self, S):
        self.S = S
        self.nc = S.nc
        self.es = ExitStack()
        self.n = 0

    def __enter__(self):
        self.es.__enter__()
        return self

    def sb(self, shape, dt=F32, name=None):
        Phase.uid += 1
        t = self.es.enter_context(self.nc.sbuf_tensor("t%d" % Phase.uid, list(shape), dt))
        return t, Buf()

    def ps(self, shape=(128, 512), dt=F32):
        Phase.uid += 1
        t = self.es.enter_context(self.nc.psum_tensor("p%d" % Phase.uid, list(shape), dt))
        return t, Buf()

    def __exit__(self, *a):
        if a[0] is None:
            self.S.barrier()
            self.S.emit()
        return self.es.__exit__(*a)


def wview(w2d, m0, m1):
    return w2d.rearrange("(kt p) m -> p kt m", p=128)[:, :, m0:m1]


def mm_group(S, ps_ap, pbuf, lhs_fn, rhs_fn, KT, reads):
    def fn(e):
        ins = None
        for kt in range(KT):
            ins = e.matmul(ps_ap, lhsT=lhs_fn(kt), rhs=rhs_fn(kt), start=(kt == 0), stop=(kt == KT - 1))
        return ins
    S.op("pe", fn, reads=reads, writes=(pbuf,))


def ln_feature_major(S, P, x, xb, FT, N, g_ap, b_ap, gb_buf, ones, ones_b, nfeat, psums, scratch=None):
    xbs = list(xb) if isinstance(xb, (list, tuple)) else [xb] * FT
    allx = tuple(dict.fromkeys(xbs))
    if scratch is None:
        scratch = [P.sb([128, 512], F32R), P.sb([128, 512], F32), P.sb([128, 512], F32), P.sb([128, 512], F32)]
    (sq, sqb), (mean, meanb), (rstd, rstdb), (tmp, tmpb) = scratch
    sq2, sq2b = P.sb([128, 512], F32R)
    sqs = [(sq, sqb), (sq2, sq2b)]
    inv = 1.0 / nfeat
    (pm, pmb), (pq, pqb) = psums[0], psums[1]

    def chunk(n0, nn):
        mm_group(S, pm[:, :nn], pmb, lambda kt: ones[:, :], lambda kt: x[:, kt, n0:n0 + nn], FT, allx + (ones_b,))
        for ft in range(FT):
            q_, q_b = sqs[ft % 2]
            S.op("act", lambda e, ft=ft, q_=q_: e.activation(out=q_[:, :nn], in_=x[:, ft, n0:n0 + nn].bitcast(F32), func=AF.Square),
                 reads=(xbs[ft],), writes=(q_b,))
            S.op("pe", lambda e, ft=ft, q_=q_: e.matmul(pq[:, :nn], lhsT=ones[:, :], rhs=q_[:, :nn], start=(ft == 0), stop=(ft == FT - 1)),
                 reads=(q_b, ones_b), writes=() if ft else (pqb,), accw=(pqb,) if ft else ())
        S.op("act", lambda e: e.activation(out=mean[:, :nn], in_=pm[:, :nn], func=AF.Identity, scale=inv), reads=(pmb,), writes=(meanb,))
        S.op("dve", lambda e: e.tensor_tensor(out=tmp[:, :nn], in0=mean[:, :nn], in1=mean[:, :nn], op=ALU.mult), reads=(meanb,), writes=(tmpb,))
        S.op("dve", lambda e: e.scalar_tensor_tensor(out=tmp[:, :nn], in0=pq[:, :nn], scalar=inv, in1=tmp[:, :nn], op0=ALU.mult, op1=ALU.subtract),
             reads=(pqb, tmpb), writes=(tmpb,))
        S.op("dve", lambda e: e.tensor_scalar(out=tmp[:, :nn], in0=tmp[:, :nn], scalar1=LN_EPS, scalar2=None, op0=ALU.add), reads=(tmpb,), writes=(tmpb,))
        S.op("act", lambda e: e.activation(out=tmp[:, :nn], in_=tmp[:, :nn], func=AF.Sqrt), reads=(tmpb,), writes=(tmpb,))
        S.op("dve", lambda e: e.reciprocal(out=rstd[:, :nn], in_=tmp[:, :nn]), reads=(tmpb,), writes=(rstdb,))
        S.op("dve", lambda e: e.scalar_tensor_tensor(out=mean[:, :nn], in0=mean[:, :nn], scalar=-1.0, in1=rstd[:, :nn], op0=ALU.mult, op1=ALU.mult),
             reads=(meanb, rstdb), writes=(meanb,))
        for ft in range(FT):
            xs = x[:, ft, n0:n0 + nn]
            S.op("dve", lambda e, xs=xs: e.tensor_tensor(out=xs, in0=xs.bitcast(F32), in1=rstd[:, :nn], op=ALU.mult), reads=(xbs[ft], rstdb), writes=(xbs[ft],))
            S.op("pool", lambda e, xs=xs: e.tensor_tensor(out=xs, in0=xs.bitcast(F32), in1=mean[:, :nn], op=ALU.add), reads=(xbs[ft], meanb), writes=(xbs[ft],))
            S.op("act", lambda e, xs=xs, ft=ft: e.activation(out=xs, in_=xs.bitcast(F32), func=AF.Identity, scale=g_ap(ft), bias=b_ap(ft)),
                 reads=(xbs[ft], gb_buf), writes=(xbs[ft],))

    for n0 in range(0, N, 512):
        chunk(n0, min(512, N - n0))


PV_BIN, PV_HYW, PV_HYB, PV_CFW, PV_CFB, PV_CFG, PV_CFBE = 0, 96, 150, 168, 354, 360, 366
PV_BO, PV_L1G, PV_L1B, PV_L2G, PV_L2B, PV_HF = 372, 388, 404, 420, 436, 452
PV_L = 456
PV_G0 = DEPTH * PV_L
PV_N = PV_G0 + 32


def cols(v):
    v = np.asarray(v, np.float32).reshape(-1)
    n = (v.size + 127) // 128
    o = np.zeros((n * 128,), np.float32)
    o[:v.size] = v
    return o.reshape(n, 128).T


def pack_pv(inp, nl):
    pv = np.zeros((128, PV_N), np.float32)
    for l in range(nl):
        b = l * PV_L
        pv[:, b + PV_BIN:b + PV_BIN + 96] = cols(inp['b_in'][l])
        for j in range(3):
            pv[:, b + PV_HYW + j * 18:b + PV_HYW + (j + 1) * 18] = cols(inp['hy_conv_w'][l, j])
        pv[:, b + PV_HYB:b + PV_HYB + 18] = cols(inp['hy_conv_b'][l])
        for j in range(31):
            pv[:, b + PV_CFW + j * 6:b + PV_CFW + (j + 1) * 6] = cols(inp['cf_dw_w'][l, j])
        pv[:, b + PV_CFB:b + PV_CFB + 6] = cols(inp['cf_dw_b'][l])
        pv[:, b + PV_CFG:b + PV_CFG + 6] = cols(inp['cf_ln_g'][l])
        pv[:, b + PV_CFBE:b + PV_CFBE + 6] = cols(inp['cf_ln_b'][l])
        pv[:, b + PV_BO:b + PV_BO + 16] = cols(inp['b_o'][l])
        pv[:, b + PV_L1G:b + PV_L1G + 16] = cols(inp['ln1_g'][l])
        pv[:, b + PV_L1B:b + PV_L1B + 16] = cols(inp['ln1_b'][l])
        pv[:, b + PV_L2G:b + PV_L2G + 16] = cols(inp['ln2_g'][l])
        pv[:, b + PV_L2B:b + PV_L2B + 16] = cols(inp['ln2_b'][l])
        for j, k in enumerate(('hy_f_b1', 'hy_f_b2', 'hy_f_b3', 'hy_f_freq')):
            pv[:64, b + PV_HF + j] = inp[k][l]
    pv[:, PV_G0:PV_G0 + 16] = cols(inp['in_ln_g'])
    pv[:, PV_G0 + 16:PV_G0 + 32] = cols(inp['in_ln_b'])
    return pv


def round_f32r(a):
    u = np.ascontiguousarray(a, np.float32).view(np.uint32)
    u = (u + np.uint32(0x800)) & np.uint32(0xFFFFF000)
    return u.view(np.float32)


_CONSTS = None


def host_consts():
    global _CONSTS
    if _CONSTS is not None:
        return _CONSTS
    L = T
    N = 2 * L
    t = np.arange(L, dtype=np.float64)
    f = np.arange(L, dtype=np.float64) + 0.5
    ang = 2.0 * np.pi * np.outer(t, f) / N
    C = np.cos(ang)
    Sm = -np.sin(ang)
    cmat = round_f32r(C.astype(np.float32))
    smat = round_f32r(Sm.astype(np.float32))
    gmat = round_f32r(np.concatenate([C.T, Sm.T], 0).astype(np.float32) * np.float32(2.0 / N))
    f32 = np.float32
    tt = np.linspace(0.0, 1.0, L, dtype=f32)[:, None]
    bands = 16
    w = (2.0 * math.pi * np.arange(L, dtype=f32)[:, None] / L).astype(f32)
    fb = np.linspace(1e-4, bands - 1, bands, dtype=f32)[None, :]
    z = np.concatenate([tt, np.cos(fb * w), -np.sin(fb * w)], axis=-1).astype(f32)
    max_decay = math.log(1e-2) / 0.3
    min_decay = math.log(1e-2) / 1.5
    deltas = np.abs(np.linspace(min_decay, max_decay, 3072, dtype=f32))
    dec = np.exp(-tt * deltas).astype(f32)
    qc = np.arange(64)[None, :]
    kc = np.arange(64)[:, None]
    cs = np.clip(qc - 8, 0, 48)
    valid = ((kc >= cs) & (kc < cs + 16)).astype(f32)
    mfull = np.zeros((64, 23, 64), f32)
    mint = np.zeros((64, 23, 64), f32)
    for i in range(15):
        mfull[:, i + 4, :] = valid
        if 4 <= i <= 11:
            mint[:, i + 4, :] = valid
    mfull, mint = pair_tab(mfull), pair_tab(mint)
    def tile_lhs(m, kt):
        return np.ascontiguousarray(m.reshape(kt, 128, m.shape[1] // 128, 128).transpose(2, 1, 0, 3)).reshape(m.shape[1] // 128, 128, kt * 128)
    gmat = tile_lhs(gmat, 32)
    _CONSTS = dict(cmat=cmat, smat=smat, gmat=gmat, cmatT=tile_lhs(cmat, 16), smatT=tile_lhs(smat, 16), zT=np.ascontiguousarray(z.T), dec=dec,
                   mfull=mfull, mint=mint, ident=np.concatenate([np.eye(128, dtype=f32), np.ones((128, 128), f32)], 1))
    return _CONSTS


def pair_tab(a):
    out = np.zeros(a.shape[:-3] + (128, 23, 64), np.float32)
    out[..., :64, :, :] = a
    out[..., 64:, 1:, :] = a[..., :, :-1, :]
    return out


def expand_rpb(rpb):
    nl = rpb.shape[0]
    qc = np.arange(64)[None, :]
    kc = np.arange(64)[:, None]
    co = np.clip(kc - qc, -15, 15) + 15
    out = np.zeros((nl, 12, 64, 23, 64), np.float32)
    for i in range(15):
        ro = 14 - i
        out[:, :, :, i + 4, :] = rpb[:, :, ro][:, :, co]
    return pair_tab(out)


class Prog:
    def __init__(self, nl, dbg=(), skip=(), ext_in=()):
        self.nl = nl
        nc = self.nc = bass.Bass("TRN2", target_bir_lowering=False)
        nc.dge_precook = False
        self.S = Sched(nc)
        self.dr = {}
        self.db = {}
        self.dbg = dbg

        def inp(name, shape, dt=F32R):
            if name in skip:
                return
            self.dr[name] = nc.dram_tensor(name, list(shape), dt, kind="ExternalInput").ap()
            self.db[name] = Buf(name)

        def scr(name, shape, dt=F32R):
            kind = "ExternalOutput" if name in dbg else ("ExternalInput" if name in ext_in else "Internal")
            self.dr[name] = nc.dram_tensor(name, list(shape), dt, kind=kind).ap()
            self.db[name] = Buf(name)
        inp("xT", [D, T])
        inp("pv", [128, PV_N], F32)
        inp("w_in", [nl, D, CIN])
        inp("rpbT", [nl, 12, 128, 23 * 64], F32)
        inp("hy_f_w1", [nl, 33, 64], F32)
        inp("hy_f_w2", [nl, 64, 64], F32)
        inp("hy_f_w3", [nl, 64, 64], F32)
        inp("hy_f_w4", [nl, 64, 3072], F32)
        inp("hy_skip", [nl, 2, 768], F32)
        inp("w_attn_br", [nl, 768, D])
        inp("w_hy_br", [nl, 768, D])
        inp("w_cf_br", [nl, 768, D])
        inp("w_o", [nl, D, D])
        inp("w_router", [D, 16])
        inp("b_router", [1, 16], F32)
        inp("moe_w_gate", [nl, NEXP, 8, 128, 16 * 128])
        inp("moe_w_up", [nl, NEXP, 8, 128, 16 * 128])
        inp("moe_w_down", [nl, NEXP, 2, 8, 128, 4 * 256])
        inp("cmat", [T, T])
        inp("smat", [T, T])
        inp("gmat", [16, 128, 32 * 128])
        inp("cmatT", [16, 128, 16 * 128])
        inp("smatT", [16, 128, 16 * 128])
        inp("zT", [33, T], F32)
        inp("dec", [T, 3072], F32)
        inp("mfull", [128, 23 * 64], F32)
        inp("mint", [128, 23 * 64], F32)
        inp("ident", [128, 256])
        scr("hTa", [D, T])
        scr("hTb", [D, T])
        scr("pT", [CIN, T])
        scr("yaT", [768, T])
        scr("yhT", [768, T])
        scr("ycT", [768, T])
        scr("mT", [D, T])
        scr("utok", [3, T, 768])
        scr("hsd", [4, T, 768])
        scr("Hsp", [4, T, 768], F32)
        scr("Ysp", [2 * T, 768])
        scr("z2", [T, 768])
        scr("yhtok", [T, 768])
        scr("cwT", [16, T], F32)
        self.dr["outT"] = nc.dram_tensor("outT", [D, T], F32R, kind="ExternalOutput").ap()
        self.db["outT"] = Buf("outT")
        self.pv = nc.alloc_sbuf_tensor("pv_sb", [128, PV_N], F32)
        self.pvb = Buf()
        self.io = nc.alloc_sbuf_tensor("ident_sb", [128, 256], F32R)
        self.identb = Buf()
        self.onesb = self.identb
        self.ident = self.io[:, 0:128]
        self.ones = self.io[:, 128:256]
        S = self.S
        S.load(self.pv[:], self.dr["pv"], self.pvb, self.db["pv"])
        S.load(self.io[:], self.dr["ident"], self.identb, self.db["ident"])
        S.barrier()
        S.emit()

    def pvc(self, col, n=1, parts=128):
        return self.pv[0:parts, col:col + n]

    def ln_block(self, src, dst, gcol, bcol, pre=None):
        S = self.S
        for tb in range(2):
            with Phase(S) as P:
                x, xb0 = P.sb([128, 16, 1024], F32R)
                xb = [xb0] + [Buf() for _ in range(15)]
                psums = [P.ps(), P.ps()]
                S.dma(x[:], wview(self.dr[src], tb * 1024, tb * 1024 + 1024), xb0, reads=(self.db[src],), writes=tuple(xb), split=2)
                ln_feature_major(S, P, x, xb, 16, 1024, lambda ft: self.pvc(gcol + ft), lambda ft: self.pvc(bcol + ft),
                                 self.pvb, self.ones, self.onesb, D, psums)
                S.dma(wview(self.dr[dst], tb * 1024, tb * 1024 + 1024), x[:], xb0, reads=tuple(xb), accw=(self.db[dst],), split=2, q="pool")

    def in_proj(self, l, hsrc):
        S = self.S
        w = self.dr["w_in"][l]
        pb = l * PV_L
        for tb in range(2):
            with Phase(S) as P:
                x, xb = P.sb([128, 16, 1024], F32R)
                wt = [P.sb([128, 16, 512], F32R) for _ in range(2)]
                ot = [P.sb([128, 512], F32R) for _ in range(3)]
                pss = [P.ps() for _ in range(4)]
                S.load(x[:], wview(self.dr[hsrc], tb * 1024, tb * 1024 + 1024), xb, self.db[hsrc])
                S.load(wt[0][0][:], wview(w, 0, 512), wt[0][1], self.db["w_in"])
                cnt = 0
                for mc in range(24):
                    wc, wcb = wt[mc % 2]
                    if mc + 1 < 24:
                        S.load(wt[(mc + 1) % 2][0][:], wview(w, (mc + 1) * 512, (mc + 2) * 512), wt[(mc + 1) % 2][1], self.db["w_in"])
                    for mi in range(4):
                        mt = mc * 4 + mi
                        for th in range(2):
                            ps, psb = pss[cnt % 4]
                            o, ob = ot[cnt % 3]
                            cnt += 1
                            mm_group(S, ps[:], psb, lambda kt, wc=wc, mi=mi: wc[:, kt, mi * 128:(mi + 1) * 128],
                                     lambda kt, th=th: x[:, kt, th * 512:(th + 1) * 512], 16, (wcb, xb))
                            S.op("act", lambda e, o=o, ps=ps, mt=mt: e.activation(out=o[:], in_=ps[:], func=AF.Identity,
                                                                                 bias=self.pvc(pb + PV_BIN + mt), scale=1.0),
                                 reads=(psb, self.pvb), writes=(ob,))
                            t0 = tb * 1024 + th * 512
                            S.store(self.dr["pT"][mt * 128:(mt + 1) * 128, t0:t0 + 512], o[:], ob, self.db["pT"])

    def attention(self, l):
        S = self.S
        pT, pTb = self.dr["pT"], self.db["pT"]
        groups = [(0, 4, list(range(0, 8, 2)), True)]
        for r0 in (4, 12, 20):
            groups.append((r0, r0 + 8, list(range(r0 - 4, r0 + 12, 2)), False))
        groups.append((28, 32, list(range(24, 32, 2)), True))
        with Phase(S) as P:
            mf, mfb = P.sb([128, 23 * 64], F32)
            mi, mib = P.sb([128, 23 * 64], F32)
            S.load(mf[:], self.dr["mfull"], mfb, self.db["mfull"], split=None)
            S.load(mi[:], self.dr["mint"], mib, self.db["mint"], split=None)
            hb = []
            for _ in range(2):
                hb.append(dict(q=P.sb([64, T], F32R), k=P.sb([64, T], F32R), vT=P.sb([64, T], F32R), v=P.sb([128, 16 * 64], F32R),
                               rp=P.sb([128, 23 * 64], F32), ef=P.sb([128, 23 * 64], F32), ei=P.sb([128, 23 * 64], F32)))
            es = [P.sb([128, 512], F32) for _ in range(4)]
            pp = [P.sb([128, 512], F32R) for _ in range(4)]
            rd, rdb = P.sb([64, 512], F32)
            ot = [P.sb([64, 512], F32R) for _ in range(2)]
            pss = [P.ps() for _ in range(4)]
            pos = [P.ps() for _ in range(2)]
            pds = [P.ps() for _ in range(2)]
            ident, ones = self.ident, self.ones
            cnt = [0, 0]

            def prep(h):
                B = hb[h % 2]
                (q, qb), (k, kb), (vT, vTb), (v, vb), (rp, rpb_), (ef, efb), (ei, eib) = (B[n] for n in ("q", "k", "vT", "v", "rp", "ef", "ei"))
                S.load(q[:], pT[O_Q + h * 64:O_Q + (h + 1) * 64, :], qb, pTb, split=None)
                S.load(k[:], pT[O_K + h * 64:O_K + (h + 1) * 64, :], kb, pTb, split=None)
                S.load(vT[:], pT[O_V + h * 64:O_V + (h + 1) * 64, :], vTb, pTb, split=None)
                S.load(rp[:], self.dr["rpbT"][l, h], rpb_, self.db["rpbT"], split=None)
                S.op("act", lambda e: e.activation(out=rp[:], in_=rp[:], func=AF.Exp), reads=(rpb_,), writes=(rpb_,))
                S.op("dve", lambda e: e.tensor_tensor(out=ef[:], in0=rp[:], in1=mf[:], op=ALU.mult), reads=(rpb_, mfb), writes=(efb,))
                S.op("pool", lambda e: e.tensor_tensor(out=ei[:], in0=rp[:], in1=mi[:], op=ALU.mult), reads=(rpb_, mib), writes=(eib,))

            def vtrans(h):
                B = hb[h % 2]
                (vT, vTb), (v, vb) = B["vT"], B["v"]
                for bnk in range(2):
                    ps, psb = pss[bnk]

                    def tr(e, ps=ps, bnk=bnk):
                        ins = None
                        for j in range(8):
                            pr = bnk * 8 + j
                            ins = e.matmul(ps[:, j * 64:(j + 1) * 64], lhsT=vT[:, pr * 128:(pr + 1) * 128], rhs=ident[0:64, 0:64], start=True, stop=True)
                        return ins
                    S.op("pe", tr, reads=(vTb, self.identb), writes=(psb,))
                    S.op("act", lambda e, ps=ps, bnk=bnk: e.activation(out=v[:, bnk * 512:(bnk + 1) * 512], in_=ps[:], func=AF.Identity),
                         reads=(psb,), writes=() if bnk else (vb,), accw=(vb,) if bnk else ())

            def front(h, st):
                B = hb[h % 2]
                (q, qb), (k, kb) = B["q"], B["k"]
                gi, r0, nq, kr, i, nk, full = st
                tab, tabb = B["ef"] if full else B["ei"]
                c = cnt[0]
                cnt[0] += 1
                ps, psb = pss[c % 4]
                e_s, e_sb = es[c % 4]
                p_, p_b = pp[c % 4]
                i0 = (r0 - kr + 7) + 4
                S.op("pe", lambda e: e.matmul(ps[:, :nq], lhsT=k[:, kr * 64:kr * 64 + 128], rhs=q[:, r0 * 64:r0 * 64 + nq], start=True, stop=True),
                     reads=(kb, qb), writes=(psb,))
                S.op("act", lambda e: e.activation(out=e_s[:, :nq], in_=ps[:, :nq], func=AF.Exp, scale=0.125), reads=(psb,), writes=(e_sb,))
                S.op("dve", lambda e: e.tensor_tensor(out=p_[:, :nq], in0=e_s[:, :nq], in1=tab[:, i0 * 64:i0 * 64 + nq], op=ALU.mult),
                     reads=(e_sb, tabb), writes=(p_b,))
                return (p_, p_b)

            def back(h, st, pbuf):
                B = hb[h % 2]
                (v, vb) = B["v"]
                gi, r0, nq, kr, i, nk, full = st
                p_, p_b = pbuf
                po, pob = pos[gi % 2]
                pd, pdb = pds[gi % 2]
                first, last = (i == 0), (i == nk - 1)
                pr = kr // 2
                S.op("pe", lambda e: e.matmul(po[0:64, :nq], lhsT=v[:, pr * 64:(pr + 1) * 64], rhs=p_[:, :nq], start=first, stop=last),
                     reads=(p_b, vb), writes=(pob,) if first else (), accw=() if first else (pob,))
                S.op("pe", lambda e: e.matmul(pd[0:64, :nq], lhsT=ones[:, 0:64], rhs=p_[:, :nq], start=first, stop=last),
                     reads=(p_b, self.onesb), writes=(pdb,) if first else (), accw=() if first else (pdb,))
                if last:
                    o, ob = ot[cnt[1] % 2]
                    cnt[1] += 1
                    S.op("act", lambda e: e.activation(out=rd[:, :nq], in_=pd[0:64, :nq], func=AF.Ln), reads=(pdb,), writes=(rdb,))
                    S.op("act", lambda e: e.activation(out=rd[:, :nq], in_=rd[:, :nq], func=AF.Exp, scale=-1.0), reads=(rdb,), writes=(rdb,))
                    S.op("dve", lambda e: e.tensor_tensor(out=o[:, :nq], in0=po[0:64, :nq], in1=rd[:, :nq], op=ALU.mult), reads=(pob, rdb), writes=(ob,))
                    S.store(self.dr["yaT"][h * 64:(h + 1) * 64, r0 * 64:r0 * 64 + nq], o[:, :nq], ob, self.db["yaT"], split=None)

            def head(h):
                steps = []
                for gi, (r0, r1, krs, full) in enumerate(groups):
                    for i, kr in enumerate(krs):
                        steps.append((gi + 5 * h, r0, (r1 - r0) * 64, kr, i, len(krs), full))
                pend = {}
                LA = 2
                for s_ in range(min(LA, len(steps))):
                    pend[s_] = front(h, steps[s_])
                for s_ in range(len(steps)):
                    if s_ + LA < len(steps):
                        pend[s_ + LA] = front(h, steps[s_ + LA])
                    back(h, steps[s_], pend.pop(s_))

            prep(0)
            for h in range(12):
                vtrans(h)
                if h + 1 < 12:
                    prep(h + 1)
                head(h)

    def conformer(self, l):
        S = self.S
        pT, pTb = self.dr["pT"], self.db["pT"]
        pb = l * PV_L
        with Phase(S) as P:
            zc, zcb0 = P.sb([128, 6, T], F32R)
            zcbs = [zcb0] + [Buf() for _ in range(5)]
            a, ab = P.sb([128, T], F32R)
            g, gb = P.sb([128, T], F32R)
            zps = [P.sb([128, T + 30], F32R) for _ in range(2)]
            dgs = [P.sb([128, 31, 128], F32R) for _ in range(2)]
            psums = [P.ps(), P.ps()]
            cps = [P.ps() for _ in range(4)]
            for zp, zpb in zps:
                S.op("dve", lambda e, zp=zp: e.tensor_scalar(out=zp[:, 0:15], in0=self.ident[:, 0:15].bitcast(F32), scalar1=0.0, scalar2=None, op0=ALU.mult),
                     reads=(self.identb,), accw=(zpb,))
                S.op("dve", lambda e, zp=zp: e.tensor_scalar(out=zp[:, 15 + T:30 + T], in0=self.ident[:, 0:15].bitcast(F32), scalar1=0.0, scalar2=None, op0=ALU.mult),
                     reads=(self.identb,), accw=(zpb,))
            cnt = [0]

            def ctile(ct):
                zp, zpb = zps[ct % 2]
                dg, dgb = dgs[ct % 2]
                zcb = zcbs[ct]
                S.load(a[:], pT[O_CF + ct * 128:O_CF + (ct + 1) * 128, :], ab, pTb, split=None)
                S.load(g[:], pT[O_CF + 768 + ct * 128:O_CF + 768 + (ct + 1) * 128, :], gb, pTb, split=None)
                for j in range(31):
                    S.op("pool", lambda e, j=j: e.tensor_scalar(out=dg[:, j, :], in0=self.ident.bitcast(F32), scalar1=self.pvc(pb + PV_CFW + j * 6 + ct), scalar2=None,
                                                                op0=ALU.mult),
                         reads=(self.identb, self.pvb), writes=(dgb,) if j == 0 else (), accw=() if j == 0 else (dgb,))
                S.op("act", lambda e: e.activation(out=g[:].bitcast(F32), in_=g[:].bitcast(F32), func=AF.Sigmoid), reads=(gb,), writes=(gb,))
                S.op("dve", lambda e: e.tensor_tensor(out=zp[:, 15:15 + T], in0=a[:].bitcast(F32), in1=g[:].bitcast(F32), op=ALU.mult),
                     reads=(ab, gb), accw=(zpb,))
                for tq in range(4):
                    ps, psb = cps[cnt[0] % 4]
                    cnt[0] += 1
                    mm_group(S, ps[:], psb, lambda j: dg[:, j, :], lambda j, tq=tq: zp[:, tq * 512 + j:tq * 512 + j + 512], 31, (dgb, zpb))
                    S.op("act", lambda e, ps=ps, tq=tq: e.activation(out=zc[:, ct, tq * 512:(tq + 1) * 512], in_=ps[:], func=AF.Identity,
                                                                     bias=self.pvc(pb + PV_CFB + ct), scale=1.0),
                         reads=(psb, self.pvb), writes=(zcb,) if tq == 0 else (), accw=() if tq == 0 else (zcb,))
            for ct in range(6):
                ctile(ct)
            ln_feature_major(S, P, zc, zcbs, 6, T, lambda ft: self.pvc(pb + PV_CFG + ft), lambda ft: self.pvc(pb + PV_CFBE + ft),
                             self.pvb, self.ones, self.onesb, 768, psums)
            for ct in range(6):
                S.op("act", lambda e, ct=ct: e.activation(out=zc[:, ct, :], in_=zc[:, ct, :].bitcast(F32), func=AF.Silu), reads=(zcbs[ct],), writes=(zcbs[ct],))
            S.dma(wview(self.dr["ycT"], 0, T), zc[:], zcb0, reads=tuple(zcbs), accw=(self.db["ycT"],), split=2, q="pool")

    def hyena(self, l):
        self.hy_conv3(l)
        self.hy_filter(l)
        self.hy_spectra()
        ut = self.dr["utok"]
        self.hy_fwd(0, ut[0], "utok")
        self.hy_inv(l, 0, ut[0], "utok", ut[1], self.dr["z2"], "z2")
        self.hy_fwd(1, self.dr["z2"], "z2")
        self.hy_inv(l, 1, self.dr["z2"], "z2", ut[2], self.dr["yhtok"], "yhtok")
        self.hy_out()

    def hy_conv3(self, l):
        S = self.S
        pT, pTb = self.dr["pT"], self.db["pT"]
        pb = l * PV_L
        with Phase(S) as P:
            us = [P.sb([128, T], F32R) for _ in range(2)]
            ucs = [P.sb([128, T], F32R) for _ in range(2)]
            ots = [P.sb([128, 4, 128], F32R) for _ in range(2)]
            pss = [P.ps() for _ in range(2)]
            cnt = [0]

            def ctile(ct):
                u, ub = us[ct % 2]
                uc, ucb = ucs[ct % 2]
                S.load(u[:], pT[O_HY + ct * 128:O_HY + (ct + 1) * 128, :], ub, pTb, split=None)
                S.op("act", lambda e: e.activation(out=uc[:], in_=u[:].bitcast(F32), func=AF.Identity, scale=self.pvc(pb + PV_HYW + 18 + ct),
                                                   bias=self.pvc(pb + PV_HYB + ct)), reads=(ub, self.pvb), writes=(ucb,))
                S.op("dve", lambda e: e.scalar_tensor_tensor(out=uc[:, 1:T], in0=u[:, 0:T - 1].bitcast(F32), scalar=self.pvc(pb + PV_HYW + ct),
                                                             in1=uc[:, 1:T].bitcast(F32), op0=ALU.mult, op1=ALU.add),
                     reads=(ub, ucb, self.pvb), writes=(ucb,))
                S.op("dve", lambda e: e.scalar_tensor_tensor(out=uc[:, 0:T - 1], in0=u[:, 1:T].bitcast(F32), scalar=self.pvc(pb + PV_HYW + 36 + ct),
                                                             in1=uc[:, 0:T - 1].bitcast(F32), op0=ALU.mult, op1=ALU.add),
                     reads=(ub, ucb, self.pvb), writes=(ucb,))
                s, c0 = ct // 6, (ct % 6) * 128
                dst = self.dr["utok"][s].rearrange("(tt p) c -> p tt c", p=128)
                for tq in range(4):
                    ps, psb = pss[cnt[0] % 2]
                    o, ob = ots[cnt[0] % 2]
                    cnt[0] += 1

                    def tr(e, ps=ps, tq=tq):
                        ins = None
                        for j in range(4):
                            tt = tq * 4 + j
                            ins = e.matmul(ps[:, j * 128:(j + 1) * 128], lhsT=uc[:, tt * 128:(tt + 1) * 128], rhs=self.ident, start=True, stop=True)
                        return ins
                    S.op("pe", tr, reads=(ucb, self.identb), writes=(psb,))
                    S.op("act", lambda e, ps=ps, o=o: e.activation(out=o[:].rearrange("p a b -> p (a b)"), in_=ps[:], func=AF.Identity),
                         reads=(psb,), writes=(ob,))
                    S.store(dst[:, tq * 4:(tq + 1) * 4, c0:c0 + 128], o[:], ob, self.db["utok"], split=None)
            for ct in range(18):
                ctile(ct)

    def hy_filter(self, l):
        S = self.S
        pb = l * PV_L + PV_HF
        with Phase(S) as P:
            zT, zTb = P.sb([33, T], F32)
            w1, w1b = P.sb([33, 64], F32)
            w2, w2b = P.sb([64, 64], F32)
            w3, w3b = P.sb([64, 64], F32)
            w4, w4b = P.sb([64, 3072], F32)
            hs_ = [P.sb([64, T], F32) for _ in range(2)]
            frb, frbb = P.sb([64, 4], F32)
            tt_, ttb = P.sb([64, 512], F32)
            mm_, mmb = P.sb([64, 512], F32)
            d0s = [P.sb([128, 384], F32) for _ in range(2)]
            d1s = [P.sb([128, 384], F32) for _ in range(2)]
            a0, a0b = P.sb([128, 384], F32)
            a1, a1b = P.sb([128, 384], F32)
            hss = [P.sb([128, 384], F32R) for _ in range(2)]
            hds = [P.sb([128, 384], F32R) for _ in range(2)]
            pss = [P.ps() for _ in range(4)]
            S.load(zT[:], self.dr["zT"], zTb, self.db["zT"], split=None)
            S.load(w1[:], self.dr["hy_f_w1"][l], w1b, self.db["hy_f_w1"], split=None)
            S.load(w2[:], self.dr["hy_f_w2"][l], w2b, self.db["hy_f_w2"], split=None)
            S.load(w3[:], self.dr["hy_f_w3"][l], w3b, self.db["hy_f_w3"], split=None)
            S.load(w4[:], self.dr["hy_f_w4"][l], w4b, self.db["hy_f_w4"], split=None)
            fr = self.pvc(pb + 3, 1, 64)
            for k in range(3):
                S.op("dve", lambda e, k=k: e.tensor_tensor(out=frb[:, k:k + 1], in0=self.pvc(pb + k, 1, 64), in1=fr, op=ALU.mult),
                     reads=(self.pvb,), writes=(frbb,))
            cur, curb, kd = zT, zTb, 33
            cnt = [0]
            for k, (w, wb) in enumerate(((w1, w1b), (w2, w2b), (w3, w3b))):
                nxt, nxtb = hs_[k % 2]

                def stage(c, cur=cur, curb=curb, kd=kd, w=w, wb=wb, nxt=nxt, nxtb=nxtb, k=k):
                    ps, psb = pss[cnt[0] % 4]
                    cnt[0] += 1
                    S.op("pe", lambda e: e.matmul(ps[0:64, :], lhsT=w[0:kd, :], rhs=cur[0:kd, c * 512:(c + 1) * 512], start=True, stop=True),
                         reads=(wb, curb), writes=(psb,))
                    S.op("dve", lambda e: e.tensor_scalar(out=tt_[:], in0=ps[0:64, :], scalar1=fr, scalar2=frb[:, k:k + 1], op0=ALU.mult, op1=ALU.add),
                         reads=(psb, frbb, self.pvb), writes=(ttb,))
                    S.op("dve", lambda e: e.tensor_scalar(out=mm_[:], in0=tt_[:], scalar1=PI, scalar2=-2.0 * PI, op0=ALU.is_gt, op1=ALU.mult),
                         reads=(ttb,), writes=(mmb,))
                    S.op("dve", lambda e: e.tensor_tensor(out=tt_[:], in0=tt_[:], in1=mm_[:], op=ALU.add), reads=(ttb, mmb), writes=(ttb,))
                    S.op("dve", lambda e: e.tensor_scalar(out=mm_[:], in0=tt_[:], scalar1=-PI, scalar2=2.0 * PI, op0=ALU.is_lt, op1=ALU.mult),
                         reads=(ttb,), writes=(mmb,))
                    S.op("dve", lambda e: e.tensor_tensor(out=tt_[:], in0=tt_[:], in1=mm_[:], op=ALU.add), reads=(ttb, mmb), writes=(ttb,))
                    S.op("act", lambda e: e.activation(out=nxt[:, c * 512:(c + 1) * 512], in_=tt_[:], func=AF.Sin), reads=(ttb,), writes=(nxtb,))
                for c in range(4):
                    stage(c)
                cur, curb, kd = nxt, nxtb, 64
            h3, h3b = cur, curb
            it = [0]

            def taps(pt, o, c2):
                i = it[0]
                it[0] += 1
                ps0, ps0b = pss[(2 * i) % 4]
                ps1, ps1b = pss[(2 * i + 1) % 4]
                d0, d0b = d0s[i % 2]
                d1, d1b = d1s[i % 2]
                hs, hsb = hss[i % 2]
                hd, hdb = hds[i % 2]
                cA = o * 768 + c2 * 384
                cB = 1536 + cA
                S.load(d0[:], self.dr["dec"][pt * 128:(pt + 1) * 128, cA:cA + 384], d0b, self.db["dec"], split=None)
                S.load(d1[:], self.dr["dec"][pt * 128:(pt + 1) * 128, cB:cB + 384], d1b, self.db["dec"], split=None)
                S.op("pe", lambda e: e.matmul(ps0[:, 0:384], lhsT=h3[:, pt * 128:(pt + 1) * 128], rhs=w4[:, cA:cA + 384], start=True, stop=True),
                     reads=(h3b, w4b), writes=(ps0b,))
                S.op("pe", lambda e: e.matmul(ps1[:, 0:384], lhsT=h3[:, pt * 128:(pt + 1) * 128], rhs=w4[:, cB:cB + 384], start=True, stop=True),
                     reads=(h3b, w4b), writes=(ps1b,))
                S.op("dve", lambda e: e.tensor_tensor(out=a0[:], in0=ps0[:, 0:384], in1=d0[:], op=ALU.mult), reads=(ps0b, d0b), writes=(a0b,))
                S.op("dve", lambda e: e.tensor_tensor(out=a1[:], in0=ps1[:, 0:384], in1=d1[:], op=ALU.mult), reads=(ps1b, d1b), writes=(a1b,))
                if pt == 0:
                    S.op("dve", lambda e: e.memset(a1[0:1, :], 0.0), reads=(a1b,), writes=(a1b,))
                S.op("dve", lambda e: e.tensor_tensor(out=hs[:], in0=a0[:], in1=a1[:], op=ALU.add), reads=(a0b, a1b), writes=(hsb,))
                S.op("dve", lambda e: e.tensor_tensor(out=hd[:], in0=a0[:], in1=a1[:], op=ALU.subtract), reads=(a0b, a1b), writes=(hdb,))
                S.store(self.dr["hsd"][o][pt * 128:(pt + 1) * 128, c2 * 384:(c2 + 1) * 384], hs[:], hsb, self.db["hsd"], split=None)
                S.store(self.dr["hsd"][2 + o][pt * 128:(pt + 1) * 128, c2 * 384:(c2 + 1) * 384], hd[:], hdb, self.db["hsd"], split=None)
            for pt in range(16):
                for o in range(2):
                    for c2 in range(2):
                        taps(pt, o, c2)

    def hy_spectra(self):
        S = self.S
        for mi_, mat in enumerate(("cmat", "smat")):
            with Phase(S) as P:
                xs = [P.sb([128, 16, 768], F32R) for _ in range(2)]
                wt = [P.sb([128, 16, 512], F32R) for _ in range(2)]
                ots = [P.sb([128, 768], F32) for _ in range(2)]
                pss = [P.ps() for _ in range(4)]
                for o in range(2):
                    S.load(xs[o][0][:], self.dr["hsd"][2 * mi_ + o].rearrange("(kt p) c -> p kt c", p=128), xs[o][1], self.db["hsd"])
                w = self.dr[mat]
                S.load(wt[0][0][:], wview(w, 0, 512), wt[0][1], self.db[mat])
                cnt = [0, 0]
                for mc in range(4):
                    wc, wcb = wt[mc % 2]
                    if mc + 1 < 4:
                        S.load(wt[(mc + 1) % 2][0][:], wview(w, (mc + 1) * 512, (mc + 2) * 512), wt[(mc + 1) % 2][1], self.db[mat])
                    for mi in range(4):
                        ft = mc * 4 + mi
                        for o in range(2):
                            x, xb = xs[o]
                            ot, otb = ots[cnt[1] % 2]
                            cnt[1] += 1
                            for (n0, nn) in ((0, 512), (512, 256)):
                                ps, psb = pss[cnt[0] % 4]
                                cnt[0] += 1
                                mm_group(S, ps[:, :nn], psb, lambda kt, wc=wc, mi=mi: wc[:, kt, mi * 128:(mi + 1) * 128],
                                         lambda kt, x=x, n0=n0, nn=nn: x[:, kt, n0:n0 + nn], 16, (wcb, xb))
                                S.op("act", lambda e, ot=ot, ps=ps, n0=n0, nn=nn: e.activation(out=ot[:, n0:n0 + nn], in_=ps[:, :nn], func=AF.Identity),
                                     reads=(psb,), writes=() if n0 else (otb,), accw=(otb,) if n0 else ())
                            S.store(self.dr["Hsp"][2 * mi_ + o][ft * 128:(ft + 1) * 128, :], ot[:], otb, self.db["Hsp"], split=None)

    def hy_fwd(self, o, zsrc, zname):
        S = self.S
        with Phase(S) as P:
            x, xb = P.sb([128, 16, 768], F32R)
            cws = [P.sb([128, 16, 128], F32R) for _ in range(2)]
            sws = [P.sb([128, 16, 128], F32R) for _ in range(2)]
            hrs = [P.sb([128, 768], F32) for _ in range(2)]
            his = [P.sb([128, 768], F32) for _ in range(2)]
            t1, t1b = P.sb([128, 384], F32)
            t2, t2b = P.sb([128, 384], F32)
            yrs = [P.sb([128, 768], F32R) for _ in range(2)]
            yis = [P.sb([128, 768], F32R) for _ in range(2)]
            pss = [P.ps() for _ in range(8)]
            S.load(x[:], zsrc.rearrange("(kt p) c -> p kt c", p=128), xb, self.db[zname])

            def ftile(ft):
                cw, cwb = cws[ft % 2]
                sw, swb = sws[ft % 2]
                hr, hrb = hrs[ft % 2]
                hi, hib = his[ft % 2]
                yr, yrb = yrs[ft % 2]
                yi, yib = yis[ft % 2]
                S.load(cw[:].rearrange("p a b -> p (a b)"), self.dr["cmatT"][ft], cwb, self.db["cmatT"], split=None)
                S.load(sw[:].rearrange("p a b -> p (a b)"), self.dr["smatT"][ft], swb, self.db["smatT"], split=None)
                S.load(hr[:], self.dr["Hsp"][o][ft * 128:(ft + 1) * 128, :], hrb, self.db["Hsp"], split=None)
                S.load(hi[:], self.dr["Hsp"][2 + o][ft * 128:(ft + 1) * 128, :], hib, self.db["Hsp"], split=None)
                for hf in range(2):
                    n0 = hf * 384
                    pre, preb = pss[(ft % 2) * 4 + hf * 2]
                    pim, pimb = pss[(ft % 2) * 4 + hf * 2 + 1]
                    mm_group(S, pre[:, :384], preb, lambda kt: cw[:, kt, :], lambda kt, n0=n0: x[:, kt, n0:n0 + 384], 16, (cwb, xb))
                    mm_group(S, pim[:, :384], pimb, lambda kt: sw[:, kt, :], lambda kt, n0=n0: x[:, kt, n0:n0 + 384], 16, (swb, xb))
                    sl = slice(n0, n0 + 384)
                    TT = lambda o_, a_, b_, op_: (lambda e: e.tensor_tensor(out=o_, in0=a_, in1=b_, op=op_))
                    S.op("dve", TT(t1[:], pre[:, :384], hr[:, sl], ALU.mult), reads=(preb, hrb), writes=(t1b,))
                    S.op("dve", TT(t2[:], pim[:, :384], hi[:, sl], ALU.mult), reads=(pimb, hib), writes=(t2b,))
                    S.op("dve", TT(yr[:, sl], t1[:], t2[:], ALU.subtract), reads=(t1b, t2b), writes=() if hf else (yrb,), accw=(yrb,) if hf else ())
                    S.op("dve", TT(t1[:], pre[:, :384], hi[:, sl], ALU.mult), reads=(preb, hib), writes=(t1b,))
                    S.op("dve", TT(t2[:], pim[:, :384], hr[:, sl], ALU.mult), reads=(pimb, hrb), writes=(t2b,))
                    S.op("dve", TT(yi[:, sl], t1[:], t2[:], ALU.add), reads=(t1b, t2b), writes=() if hf else (yib,), accw=(yib,) if hf else ())
                S.store(self.dr["Ysp"][ft * 128:(ft + 1) * 128, :], yr[:], yrb, self.db["Ysp"], split=None)
                S.store(self.dr["Ysp"][T + ft * 128:T + (ft + 1) * 128, :], yi[:], yib, self.db["Ysp"], split=None)
            for ft in range(16):
                ftile(ft)

    def hy_inv(self, l, o, zsrc, zname, xm, dst, dname):
        S = self.S
        with Phase(S) as P:
            y, yb = P.sb([128, 32, 768], F32R)
            wt = [P.sb([128, 32, 128], F32R) for _ in range(2)]
            sk, skb = P.sb([128, 768], F32)
            zs = [P.sb([128, 768], F32R) for _ in range(2)]
            xms = [P.sb([128, 768], F32R) for _ in range(2)]
            a, ab = P.sb([128, 768], F32)
            b2, b2b = P.sb([128, 384], F32)
            ots = [P.sb([128, 768], F32R) for _ in range(2)]
            pss = [P.ps() for _ in range(4)]
            S.load(y[:], self.dr["Ysp"].rearrange("(kt p) c -> p kt c", p=128), yb, self.db["Ysp"])
            S.load(sk[:], self.dr["hy_skip"][l, o].partition_broadcast(128), skb, self.db["hy_skip"], split=None)

            def ttile(tt):
                w, wb = wt[tt % 2]
                z, zb = zs[tt % 2]
                xmt, xmb = xms[tt % 2]
                ot, otb = ots[tt % 2]
                S.load(w[:].rearrange("p a b -> p (a b)"), self.dr["gmat"][tt], wb, self.db["gmat"], split=None)
                S.load(z[:], zsrc[tt * 128:(tt + 1) * 128, :], zb, self.db[zname], split=None)
                S.load(xmt[:], xm[tt * 128:(tt + 1) * 128, :], xmb, self.db["utok"], split=None)
                S.op("dve", lambda e: e.tensor_tensor(out=a[:], in0=z[:].bitcast(F32), in1=sk[:], op=ALU.mult), reads=(zb, skb), writes=(ab,))
                for hf in range(2):
                    n0 = hf * 384
                    ps, psb = pss[(tt % 2) * 2 + hf]
                    mm_group(S, ps[:, :384], psb, lambda kt: w[:, kt, :], lambda kt, n0=n0: y[:, kt, n0:n0 + 384], 32, (wb, yb))
                    S.op("dve", lambda e, ps=ps, n0=n0: e.tensor_tensor(out=b2[:], in0=ps[:, :384], in1=a[:, n0:n0 + 384], op=ALU.add),
                         reads=(psb, ab), writes=(b2b,))
                    S.op("dve", lambda e, n0=n0: e.tensor_tensor(out=ot[:, n0:n0 + 384], in0=b2[:], in1=xmt[:, n0:n0 + 384].bitcast(F32), op=ALU.mult),
                         reads=(b2b, xmb), writes=() if hf else (otb,), accw=(otb,) if hf else ())
                S.store(dst[tt * 128:(tt + 1) * 128, :], ot[:], otb, self.db[dname], split=None)
            for tt in range(16):
                ttile(tt)

    def hy_out(self):
        S = self.S
        with Phase(S) as P:
            y, yb = P.sb([128, 16, 768], F32R)
            ots = [P.sb([128, 512], F32R) for _ in range(2)]
            pss = [P.ps() for _ in range(2)]
            S.load(y[:], self.dr["yhtok"].rearrange("(kt p) c -> p kt c", p=128), yb, self.db["yhtok"])
            cnt = [0]
            for ct in range(6):
                for tq in range(4):
                    ps, psb = pss[cnt[0] % 2]
                    o, ob = ots[cnt[0] % 2]
                    cnt[0] += 1

                    def tr(e, ps=ps, tq=tq, ct=ct):
                        ins = None
                        for j in range(4):
                            ins = e.matmul(ps[:, j * 128:(j + 1) * 128], lhsT=y[:, tq * 4 + j, ct * 128:(ct + 1) * 128], rhs=self.ident, start=True, stop=True)
                        return ins
                    S.op("pe", tr, reads=(yb, self.identb), writes=(psb,))
                    S.op("act", lambda e, ps=ps, o=o: e.activation(out=o[:], in_=ps[:], func=AF.Identity), reads=(psb,), writes=(ob,))
                    S.store(self.dr["yhT"][ct * 128:(ct + 1) * 128, tq * 512:(tq + 1) * 512], o[:], ob, self.db["yhT"], split=None)

    def merge(self, l):
        S = self.S
        pT, pTb = self.dr["pT"], self.db["pT"]
        brs = (("yaT", "w_attn_br", O_GA), ("yhT", "w_hy_br", O_GH), ("ycT", "w_cf_br", O_GC))
        for tb in range(2):
            with Phase(S) as P:
                ys = [P.sb([128, 6, 1024], F32R) for _ in range(3)]
                ws = [[P.sb([128, 6, 128], F32R) for _ in range(2)] for _ in range(3)]
                gts = [P.sb([128, 512], F32R) for _ in range(3)]
                sgs = [P.sb([128, 512], F32) for _ in range(2)]
                tmp, tmpb = P.sb([128, 512], F32)
                accs = [P.sb([128, 512], F32R) for _ in range(2)]
                pss = [P.ps() for _ in range(4)]
                for i, (yn, wn, og) in enumerate(brs):
                    S.load(ys[i][0][:], wview(self.dr[yn], tb * 1024, (tb + 1) * 1024), ys[i][1], self.db[yn])
                cnt = [0, 0]

                def mtile(mt):
                    for i, (yn, wn, og) in enumerate(brs):
                        w, wb = ws[i][mt % 2]
                        S.load(w[:], wview(self.dr[wn][l], mt * 128, (mt + 1) * 128), wb, self.db[wn], split=None)
                    for th in range(2):
                        acc, accb = accs[cnt[1] % 2]
                        cnt[1] += 1
                        t0 = tb * 1024 + th * 512
                        for i, (yn, wn, og) in enumerate(brs):
                            w, wb = ws[i][mt % 2]
                            y, yb = ys[i]
                            ps, psb = pss[cnt[0] % 4]
                            sg, sgb = sgs[cnt[0] % 2]
                            gt, gtb = gts[i]
                            cnt[0] += 1
                            mm_group(S, ps[:], psb, lambda kt, w=w: w[:, kt, :], lambda kt, y=y, th=th: y[:, kt, th * 512:(th + 1) * 512], 6, (wb, yb))
                            S.load(gt[:], pT[og + mt * 128:og + (mt + 1) * 128, t0:t0 + 512], gtb, pTb, split=None)
                            S.op("act", lambda e, sg=sg, gt=gt: e.activation(out=sg[:], in_=gt[:].bitcast(F32), func=AF.Sigmoid), reads=(gtb,), writes=(sgb,))
                            if i == 0:
                                S.op("dve", lambda e, acc=acc, ps=ps, sg=sg: e.tensor_tensor(out=acc[:], in0=ps[:], in1=sg[:], op=ALU.mult),
                                     reads=(psb, sgb), writes=(accb,))
                            else:
                                S.op("dve", lambda e, ps=ps, sg=sg: e.tensor_tensor(out=tmp[:], in0=ps[:], in1=sg[:], op=ALU.mult),
                                     reads=(psb, sgb), writes=(tmpb,))
                                S.op("dve", lambda e, acc=acc: e.tensor_tensor(out=acc[:], in0=acc[:].bitcast(F32), in1=tmp[:], op=ALU.add),
                                     reads=(accb, tmpb), writes=(accb,))
                        S.store(self.dr["mT"][mt * 128:(mt + 1) * 128, t0:t0 + 512], acc[:], accb, self.db["mT"], split=None)
                for mt in range(16):
                    mtile(mt)

    def wo_ln(self, l, hsrc, hdst):
        S = self.S
        pb = l * PV_L
        w = self.dr["w_o"][l]
        for tb in range(2):
            with Phase(S) as P:
                m, mb = P.sb([128, 16, 1024], F32R)
                xn, xnb0 = P.sb([128, 16, 1024], F32R)
                xnbs = [xnb0] + [Buf() for _ in range(15)]
                wt = [P.sb([128, 16, 256], F32R) for _ in range(2)]
                hts = [P.sb([128, 512], F32R) for _ in range(2)]
                pss = [P.ps() for _ in range(4)]
                lnps = [P.ps(), P.ps()]
                S.load(m[:], wview(self.dr["mT"], tb * 1024, (tb + 1) * 1024), mb, self.db["mT"])
                S.load(wt[0][0][:], wview(w, 0, 256), wt[0][1], self.db["w_o"])
                cnt = [0]
                for mc in range(8):
                    wc, wcb = wt[mc % 2]
                    if mc + 1 < 8:
                        S.load(wt[(mc + 1) % 2][0][:], wview(w, (mc + 1) * 256, (mc + 2) * 256), wt[(mc + 1) % 2][1], self.db["w_o"])
                    for mi in range(2):
                        mt = mc * 2 + mi
                        for th in range(2):
                            ps, psb = pss[cnt[0] % 4]
                            ht, htb = hts[cnt[0] % 2]
                            cnt[0] += 1
                            t0 = tb * 1024 + th * 512
                            xs = xn[:, mt, th * 512:(th + 1) * 512]
                            mm_group(S, ps[:], psb, lambda kt, wc=wc, mi=mi: wc[:, kt, mi * 128:(mi + 1) * 128],
                                     lambda kt, th=th: m[:, kt, th * 512:(th + 1) * 512], 16, (wcb, mb))
                            S.load(ht[:], self.dr[hsrc][mt * 128:(mt + 1) * 128, t0:t0 + 512], htb, self.db[hsrc], split=None)
                            S.op("act", lambda e, xs=xs, ps=ps, mt=mt: e.activation(out=xs, in_=ps[:], func=AF.Identity, bias=self.pvc(pb + PV_BO + mt), scale=1.0),
                                 reads=(psb, self.pvb), accw=(xnbs[mt],))
                            S.op("dve", lambda e, xs=xs, ht=ht: e.scalar_tensor_tensor(out=xs, in0=ht[:].bitcast(F32), scalar=ALPHA, in1=xs.bitcast(F32),
                                                                                      op0=ALU.mult, op1=ALU.add),
                                 reads=(htb, xnbs[mt]), accw=(xnbs[mt],))
                ln_feature_major(S, P, xn, xnbs, 16, 1024, lambda ft: self.pvc(pb + PV_L1G + ft), lambda ft: self.pvc(pb + PV_L1B + ft),
                                 self.pvb, self.ones, self.onesb, D, lnps)
                S.dma(wview(self.dr[hdst], tb * 1024, (tb + 1) * 1024), xn[:], xnb0, reads=tuple(xnbs), accw=(self.db[hdst],), split=2, q="pool")

    def router(self, hsrc):
        S = self.S
        with Phase(S) as P:
            wr, wrb = P.sb([128, 16, 16], F32R)
            br, brb = P.sb([128, 16], F32)
            xs = [P.sb([128, 16, 128], F32R) for _ in range(2)]
            pss = [P.ps() for _ in range(2)]
            pts = [P.ps() for _ in range(2)]
            cts = [P.sb([16, 128], F32) for _ in range(2)]
            S.load(wr[:], self.dr["w_router"].rearrange("(kt p) e -> p kt e", p=128), wrb, self.db["w_router"], split=None)
            S.load(br[:], self.dr["b_router"][0].partition_broadcast(128), brb, self.db["b_router"], split=None)

            def tile(tt):
                x, xb = xs[tt % 2]
                ps, psb = pss[tt % 2]
                pt, ptb = pts[tt % 2]
                ct, ctb = cts[tt % 2]
                S.load(x[:], wview(self.dr[hsrc], tt * 128, (tt + 1) * 128), xb, self.db[hsrc], split=8)
                mm_group(S, ps[:, 0:16], psb, lambda kt: x[:, kt, :], lambda kt: wr[:, kt, :], 16, (xb, wrb))
                names = ("lg", "ex", "em", "sel", "m1", "m2", "sc", "gs", "mx", "s1")
                shp = dict(lg=16, ex=16, em=16, sel=16, m1=4, m2=4, sc=4, gs=4, mx=1, s1=1)
                t = {n: P.sb([128, shp[n]], F32) for n in names}
                cw, cwb = P.sb([128, 16], F32R)

                def A(n):
                    return t[n][0]

                def B(n):
                    return t[n][1]
                g3 = lambda ap: ap.rearrange("p (g e) -> p g e", g=4)
                S.op("dve", lambda e: e.tensor_tensor(out=A("lg")[:], in0=ps[:, 0:16], in1=br[:], op=ALU.add), reads=(psb, brb), writes=(B("lg"),))
                S.op("dve", lambda e: e.tensor_reduce(out=A("mx")[:], in_=A("lg")[:], axis=mybir.AxisListType.X, op=ALU.max), reads=(B("lg"),), writes=(B("mx"),))
                S.op("dve", lambda e: e.tensor_scalar(out=A("mx")[:], in0=A("mx")[:], scalar1=-1.0, scalar2=None, op0=ALU.mult), reads=(B("mx"),), writes=(B("mx"),))
                S.op("act", lambda e: e.activation(out=A("ex")[:], in_=A("lg")[:], func=AF.Exp, bias=A("mx")[:, 0:1], scale=1.0),
                     reads=(B("lg"), B("mx")), writes=(B("ex"),))
                S.op("dve", lambda e: e.tensor_reduce(out=A("m1")[:], in_=g3(A("ex")[:]), axis=mybir.AxisListType.X, op=ALU.max), reads=(B("ex"),), writes=(B("m1"),))
                for g in range(4):
                    S.op("dve", lambda e, g=g: e.tensor_scalar(out=A("em")[:, 4 * g:4 * g + 4], in0=A("ex")[:, 4 * g:4 * g + 4], scalar1=A("m1")[:, g:g + 1],
                                                               scalar2=-1e30, op0=ALU.is_equal, op1=ALU.mult),
                         reads=(B("ex"), B("m1")), writes=() if g else (B("em"),), accw=(B("em"),) if g else ())
                S.op("dve", lambda e: e.tensor_tensor(out=A("em")[:], in0=A("em")[:], in1=A("ex")[:], op=ALU.add), reads=(B("em"), B("ex")), writes=(B("em"),))
                S.op("dve", lambda e: e.tensor_reduce(out=A("m2")[:], in_=g3(A("em")[:]), axis=mybir.AxisListType.X, op=ALU.max), reads=(B("em"),), writes=(B("m2"),))
                S.op("dve", lambda e: e.tensor_tensor(out=A("sc")[:], in0=A("m1")[:], in1=A("m2")[:], op=ALU.add), reads=(B("m1"), B("m2")), writes=(B("sc"),))
                S.op("dve", lambda e: e.tensor_reduce(out=A("s1")[:], in_=A("sc")[:], axis=mybir.AxisListType.X, op=ALU.max), reads=(B("sc"),), writes=(B("s1"),))
                S.op("dve", lambda e: e.tensor_scalar(out=A("gs")[:], in0=A("sc")[:], scalar1=A("s1")[:, 0:1], scalar2=None, op0=ALU.is_equal),
                     reads=(B("sc"), B("s1")), writes=(B("gs"),))
                for g in range(4):
                    S.op("dve", lambda e, g=g: e.tensor_scalar(out=A("sel")[:, 4 * g:4 * g + 4], in0=A("ex")[:, 4 * g:4 * g + 4], scalar1=A("m2")[:, g:g + 1],
                                                               scalar2=A("gs")[:, g:g + 1], op0=ALU.is_ge, op1=ALU.mult),
                         reads=(B("ex"), B("m2"), B("gs")), writes=() if g else (B("sel"),), accw=(B("sel"),) if g else ())
                S.op("dve", lambda e: e.tensor_tensor(out=A("sel")[:], in0=A("sel")[:], in1=A("ex")[:], op=ALU.mult), reads=(B("sel"), B("ex")), writes=(B("sel"),))
                S.op("dve", lambda e: e.tensor_reduce(out=A("s1")[:], in_=A("sel")[:], axis=mybir.AxisListType.X, op=ALU.add), reads=(B("sel"),), writes=(B("s1"),))
                S.op("dve", lambda e: e.reciprocal(out=A("s1")[:], in_=A("s1")[:]), reads=(B("s1"),), writes=(B("s1"),))
                S.op("dve", lambda e: e.tensor_scalar(out=cw[:], in0=A("sel")[:], scalar1=A("s1")[:, 0:1], scalar2=None, op0=ALU.mult),
                     reads=(B("sel"), B("s1")), writes=(cwb,))
                S.op("pe", lambda e: e.matmul(pt[0:16, 0:128], lhsT=cw[:, :], rhs=self.ident, start=True, stop=True), reads=(cwb, self.identb), writes=(ptb,))
                S.op("act", lambda e: e.activation(out=ct[:], in_=pt[0:16, 0:128], func=AF.Identity), reads=(ptb,), writes=(ctb,))
                S.store(self.dr["cwT"][:, tt * 128:(tt + 1) * 128], ct[:], ctb, self.db["cwT"], split=None)
            for tt in range(16):
                tile(tt)

    def moe(self, l, hsrc, hdst):
        S = self.S
        pb = l * PV_L
        self.router(hsrc)
        TB = 1024
        for tbk in range(T // TB):
            with Phase(S) as P:
                x, xb = P.sb([128, 16, TB], F32R)
                ya, yab0 = P.sb([128, 16, TB], F32R)
                yabs = [yab0] + [Buf() for _ in range(15)]
                hs, hsb = P.sb([128, 4, TB], F32R)
                wgs = [P.sb([128, 16, 128], F32R) for _ in range(2)]
                wus = [P.sb([128, 16, 128], F32R) for _ in range(2)]
                wds = [P.sb([128, 4, 256], F32R) for _ in range(2)]
                cwb_, cwbb = P.sb([128, TB], F32)
                scr = [P.sb([128, 512], F32R), P.sb([128, 512], F32), P.sb([128, 512], F32), P.sb([128, 512], F32)]
                (tg, tgb), (tg2, tg2b) = scr[1], scr[2]
                psg = [P.ps() for _ in range(2)]
                psu = [P.ps() for _ in range(2)]
                psy = [P.ps() for _ in range(2)]
                lnps = [P.ps(), P.ps()]
                t0 = tbk * TB
                S.load(x[:], wview(self.dr[hsrc], t0, t0 + TB), xb, self.db[hsrc])
                for mt in range(16):
                    S.op("act", lambda e, mt=mt: e.activation(out=ya[:, mt, :], in_=x[:, mt, :].bitcast(F32), func=AF.Identity, scale=ALPHA),
                         reads=(xb,), writes=(yabs[mt],))
                cnt = [0, 0, 0, 0]

                def expert(e_):
                    S.load(cwb_[:], self.dr["cwT"][e_, t0:t0 + TB].partition_broadcast(128), cwbb, self.db["cwT"], split=None)
                    wgd = self.dr["moe_w_gate"][l, e_]
                    wud = self.dr["moe_w_up"][l, e_]
                    wdd = self.dr["moe_w_down"][l, e_]
                    for fh in range(2):
                        for fi in range(4):
                            ft = fh * 4 + fi
                            i = cnt[0]
                            cnt[0] += 1
                            wg, wgb = wgs[i % 2]
                            wu, wub = wus[i % 2]
                            S.load(wg[:].rearrange("p a b -> p (a b)"), wgd[ft], wgb, self.db["moe_w_gate"], split=None)
                            S.load(wu[:].rearrange("p a b -> p (a b)"), wud[ft], wub, self.db["moe_w_up"], split=None)
                            for th in range(2):
                                k_ = cnt[3]
                                cnt[3] += 1
                                pg, pgb = psg[k_ % 2]
                                pu, pub = psu[k_ % 2]
                                ts = slice(th * 512, (th + 1) * 512)
                                mm_group(S, pg[:], pgb, lambda kt, wg=wg: wg[:, kt, :], lambda kt, ts=ts: x[:, kt, ts], 16, (wgb, xb))
                                mm_group(S, pu[:], pub, lambda kt, wu=wu: wu[:, kt, :], lambda kt, ts=ts: x[:, kt, ts], 16, (wub, xb))
                                S.op("act", lambda e, pg=pg: e.activation(out=tg[:], in_=pg[:], func=AF.Silu), reads=(pgb,), writes=(tgb,))
                                S.op("pool", lambda e, ts=ts: e.tensor_tensor(out=tg2[:], in0=tg[:], in1=cwb_[:, ts], op=ALU.mult), reads=(tgb, cwbb), writes=(tg2b,))
                                first = (fi == 0 and th == 0)
                                S.op("dve", lambda e, pu=pu, fi=fi, ts=ts: e.tensor_tensor(out=hs[:, fi, ts], in0=pu[:], in1=tg2[:], op=ALU.mult),
                                     reads=(pub, tg2b), writes=(hsb,) if first else (), accw=() if first else (hsb,))
                        for dc in range(8):
                            j = cnt[1]
                            cnt[1] += 1
                            wd, wdb = wds[j % 2]
                            S.load(wd[:].rearrange("p a b -> p (a b)"), wdd[fh, dc], wdb, self.db["moe_w_down"], split=None)
                            for mi in range(2):
                                mt = dc * 2 + mi
                                for th in range(2):
                                    ts = slice(th * 512, (th + 1) * 512)
                                    py, pyb = psy[cnt[2] % 2]
                                    cnt[2] += 1
                                    mm_group(S, py[:], pyb, lambda kt, wd=wd, mi=mi: wd[:, kt, mi * 128:(mi + 1) * 128], lambda kt, ts=ts: hs[:, kt, ts], 4, (wdb, hsb))
                                    S.op("dve", lambda e, py=py, mt=mt, ts=ts: e.tensor_tensor(out=ya[:, mt, ts], in0=py[:], in1=ya[:, mt, ts].bitcast(F32), op=ALU.add),
                                         reads=(pyb, yabs[mt]), accw=(yabs[mt],))
                for e_ in range(NEXP):
                    expert(e_)
                ln_feature_major(S, P, ya, yabs, 16, TB, lambda ft: self.pvc(pb + PV_L2G + ft), lambda ft: self.pvc(pb + PV_L2B + ft),
                                 self.pvb, self.ones, self.onesb, D, lnps, scratch=scr)
                S.dma(wview(self.dr[hdst], t0, t0 + TB), ya[:], yab0, reads=tuple(yabs), accw=(self.db[hdst],), split=2, q="pool")

    def layer(self, l, last):
        self.in_proj(l, "hTa")
        self.attention(l)
        self.hyena(l)
        self.conformer(l)
        self.merge(l)
        self.wo_ln(l, "hTa", "hTb")
        self.moe(l, "hTb", "outT" if last else "hTa")

    def full(self):
        self.ln_block("xT", "hTa", PV_G0, PV_G0 + 16)
        for l in range(self.nl):
            self.layer(l, l == self.nl - 1)


def tile_gu(w, nl):
    w = np.asarray(w[:nl], np.float32)
    return np.ascontiguousarray(w.reshape(nl, NEXP, 16, 128, 8, 128).transpose(0, 1, 4, 3, 2, 5)).reshape(nl, NEXP, 8, 128, 16 * 128)


def tile_down(w, nl):
    w = np.asarray(w[:nl], np.float32)
    return np.ascontiguousarray(w.reshape(nl, NEXP, 2, 4, 128, 8, 256).transpose(0, 1, 2, 5, 4, 3, 6)).reshape(nl, NEXP, 2, 8, 128, 4 * 256)


def make_inputs(inp, nl, b):
    C = host_consts()
    m = dict(xT=np.ascontiguousarray(np.asarray(inp['x'][b], np.float32).T), pv=inp['_pv'], w_in=inp['w_in'][:nl],
             rpbT=inp['_rpbT'], hy_f_w1=inp['hy_f_w1'][:nl], hy_f_w2=inp['hy_f_w2'][:nl], hy_f_w3=inp['hy_f_w3'][:nl],
             hy_f_w4=inp['hy_f_w4'][:nl], hy_skip=inp['hy_skip'][:nl], w_attn_br=inp['w_attn_br'][:nl], w_hy_br=inp['w_hy_br'][:nl],
             w_cf_br=inp['w_cf_br'][:nl], w_o=inp['w_o'][:nl], w_router=inp['w_router'], b_router=np.asarray(inp['b_router']).reshape(1, 16),
             moe_w_gate=tile_gu(inp['moe_w_gate'], nl), moe_w_up=tile_gu(inp['moe_w_up'], nl), moe_w_down=tile_down(inp['moe_w_down'], nl),
             cmat=C['cmat'], smat=C['smat'], gmat=C['gmat'], cmatT=C['cmatT'], smatT=C['smatT'], zT=C['zT'], dec=C['dec'],
             mfull=C['mfull'].reshape(128, -1), mint=C['mint'].reshape(128, -1), ident=C['ident'])
    return m


def kernel(**inputs):
    inp = {k: np.asarray(v) for k, v in inputs.items()}
    nl = DEPTH
    inp['_pv'] = pack_pv(inp, nl)
    inp['_rpbT'] = expand_rpb(inp['attn_rpb'][:nl]).reshape(nl, 12, 128, 23 * 64)
    P = Prog(nl)
    P.full()
    shared = make_inputs(inp, nl, 0)
    in_maps = []
    for b in range(8):
        m = dict(shared)
        m['xT'] = np.ascontiguousarray(inp['x'][b].astype(np.float32).T)
        in_maps.append({k: np.ascontiguousarray(v, dtype=np.float32) if v.dtype != np.float32 or not v.flags.c_contiguous else v for k, v in m.items()})
    res = run_bass_kernel_spmd(P.nc, in_maps, core_ids=list(range(8)))
    out = np.stack([np.ascontiguousarray(res.results[b]["outT"].T) for b in range(8)], 0)
    return out.astype(np.float32)
```

```python
import math
from contextlib import ExitStack
import numpy as np
import concourse.bass as bass
import concourse.mybir as mybir
from concourse.bass_utils import run_bass_kernel_spmd

F32 = mybir.dt.float32
F32R = mybir.dt.float32r
AF = mybir.ActivationFunctionType
ALU = mybir.AluOpType

D = 2048
T = 2048
DEPTH = 4
GW = 64
MIXW = 768
CIN = 12288
NEXP = 16
DEXP = 1024
ALPHA = (2 * DEPTH) ** 0.25
LN_EPS = 1e-5
O_Q, O_K, O_V, O_HY, O_CF, O_GA, O_GH, O_GC = 0, 768, 1536, 2304, 4608, 6144, 8192, 10240
PI = math.pi


class Buf:
    def __init__(self, name=""):
        self.w = {}
        self.r = {}
        self.sem = None
        self.name = name


class Sched:
    ENGS = ("pe", "act", "dve", "pool", "sp")

    def __init__(self, nc):
        self.nc = nc
        self.eng = {"pe": nc.tensor, "act": nc.scalar, "dve": nc.vector, "pool": nc.gpsimd, "sp": nc.sync}
        self.q = {e: [] for e in self.ENGS}
        self.sems = []
        self.esem = {e: self._newsem("e_" + e) for e in ("pe", "act", "dve", "pool")}
        self.known = {e: {} for e in self.ENGS}
        self.dma_pool = []
        self.dma_next = 0
        self.nins = 0

    def _newsem(self, name):
        self.sems.append([self.nc.alloc_semaphore(name), 0])
        return len(self.sems) - 1

    def _dma_sem(self):
        if self.dma_next == len(self.dma_pool):
            self.dma_pool.append(self._newsem("d%d" % len(self.dma_pool)))
        self.dma_next += 1
        return self.dma_pool[self.dma_next - 1]

    @staticmethod
    def _deps(reads, writes, accw):
        d = {}

        def add(m):
            for k, v in m.items():
                if d.get(k, 0) < v:
                    d[k] = v
        for b in reads:
            add(b.w)
        for b in writes:
            add(b.w)
            add(b.r)
        for b in accw:
            add(b.r)
        return d

    @staticmethod
    def _commit(sid, val, reads, writes, accw):
        for b in reads:
            if b.r.get(sid, 0) < val:
                b.r[sid] = val
        for b in writes:
            b.w = {sid: val}
            b.r = {}
        for b in accw:
            if b.w.get(sid, 0) < val:
                b.w[sid] = val

    def _filter(self, eng, d):
        out = []
        kn = self.known[eng]
        for k, v in d.items():
            if kn.get(k, 0) < v:
                kn[k] = v
                out.append((k, v))
        return out

    def op(self, eng, fn, reads=(), writes=(), accw=()):
        d = self._deps(reads, writes, accw)
        sid = self.esem[eng]
        if eng == "pe":
            d.pop(sid, None)
        waits = self._filter(eng, d)
        self.sems[sid][1] += 1
        self.q[eng].append((waits, fn, sid, 1))
        self._commit(sid, self.sems[sid][1], reads, writes, accw)

    def dma(self, out, in_, sb, reads=(), writes=(), accw=(), q="sp", split=None):
        if sb.sem is None:
            sb.sem = self._dma_sem()
        sid = sb.sem
        d = self._deps(reads, writes, accw)
        waits = self._filter(q, d)
        pairs = [(out, in_)]
        if split is not None and len(out.shape) >= 3 and out.shape[1] > split:
            n1 = out.shape[1]
            pairs = [(out[:, a:min(a + split, n1)], in_[:, a:min(a + split, n1)]) for a in range(0, n1, split)]
        for i, (o, ii) in enumerate(pairs):
            self.sems[sid][1] += 16
            self.q[q].append((waits if i == 0 else [], lambda e, o=o, ii=ii: e.dma_start(out=o, in_=ii), sid, 16))
        self._commit(sid, self.sems[sid][1], reads, writes, accw)

    def load(self, tile_ap, dram_ap, sb, db, q="sp", split=2):
        self.dma(tile_ap, dram_ap, sb, reads=(db,), writes=(sb,), q=q, split=split)

    def store(self, dram_ap, tile_ap, sb, db, q="pool", split=2):
        self.dma(dram_ap, tile_ap, sb, reads=(sb,), accw=(db,), q=q, split=split)

    def barrier(self):
        allv = {i: c for i, (h, c) in enumerate(self.sems) if c > 0}
        for e in self.ENGS:
            waits = self._filter(e, dict(allv))
            if waits:
                self.q[e].append((waits, None, None, 0))

    def emit(self):
        nc = self.nc
        q = self.q
        sems = self.sems
        with nc.Block(no_gpsimd_drain=True) as blk:
            def run(e, items):
                for waits, fn, sid, inc in items:
                    for k, v in waits:
                        e.wait_ge(sems[k][0], v)
                    if fn is not None:
                        fn(e).then_inc(sems[sid][0], inc)
                        self.nins += 1

            @blk.tensor
            def _(e):
                run(e, q["pe"])

            @blk.scalar
            def _(e):
                run(e, q["act"])

            @blk.vector
            def _(e):
                run(e, q["dve"])

            @blk.gpsimd
            def _(e):
                run(e, q["pool"])

            @blk.sync
            def _(e):
                run(e, q["sp"])
        self.q = {e: [] for e in self.ENGS}
        self.dma_next = 0


class Phase:
    uid = 0

    def __init__(self, S):
        self.S = S
        self.nc = S.nc
        self.es = ExitStack()
        self.n = 0

    def __enter__(self):
        self.es.__enter__()
        return self

    def sb(self, shape, dt=F32, name=None):
        Phase.uid += 1
        t = self.es.enter_context(self.nc.sbuf_tensor("t%d" % Phase.uid, list(shape), dt))
        return t, Buf()

    def ps(self, shape=(128, 512), dt=F32):
        Phase.uid += 1
        t = self.es.enter_context(self.nc.psum_tensor("p%d" % Phase.uid, list(shape), dt))
        return t, Buf()

    def __exit__(self, *a):
        if a[0] is None:
            self.S.barrier()
            self.S.emit()
        return self.es.__exit__(*a)


def wview(w2d, m0, m1):
    return w2d.rearrange("(kt p) m -> p kt m", p=128)[:, :, m0:m1]


def mm_group(S, ps_ap, pbuf, lhs_fn, rhs_fn, KT, reads):
    def fn(e):
        ins = None
        for kt in range(KT):
            ins = e.matmul(ps_ap, lhsT=lhs_fn(kt), rhs=rhs_fn(kt), start=(kt == 0), stop=(kt == KT - 1))
        return ins
    S.op("pe", fn, reads=reads, writes=(pbuf,))


def ln_feature_major(S, P, x, xb, FT, N, g_ap, b_ap, gb_buf, ones, ones_b, nfeat, psums, scratch=None):
    xbs = list(xb) if isinstance(xb, (list, tuple)) else [xb] * FT
    allx = tuple(dict.fromkeys(xbs))
    if scratch is None:
        scratch = [P.sb([128, 512], F32R), P.sb([128, 512], F32), P.sb([128, 512], F32), P.sb([128, 512], F32)]
    (sq, sqb), (mean, meanb), (rstd, rstdb), (tmp, tmpb) = scratch
    sq2, sq2b = P.sb([128, 512], F32R)
    sqs = [(sq, sqb), (sq2, sq2b)]
    inv = 1.0 / nfeat
    (pm, pmb), (pq, pqb) = psums[0], psums[1]

    def chunk(n0, nn):
        mm_group(S, pm[:, :nn], pmb, lambda kt: ones[:, :], lambda kt: x[:, kt, n0:n0 + nn], FT, allx + (ones_b,))
        for ft in range(FT):
            q_, q_b = sqs[ft % 2]
            S.op("act", lambda e, ft=ft, q_=q_: e.activation(out=q_[:, :nn], in_=x[:, ft, n0:n0 + nn].bitcast(F32), func=AF.Square),
                 reads=(xbs[ft],), writes=(q_b,))
            S.op("pe", lambda e, ft=ft, q_=q_: e.matmul(pq[:, :nn], lhsT=ones[:, :], rhs=q_[:, :nn], start=(ft == 0), stop=(ft == FT - 1)),
                 reads=(q_b, ones_b), writes=() if ft else (pqb,), accw=(pqb,) if ft else ())
        S.op("act", lambda e: e.activation(out=mean[:, :nn], in_=pm[:, :nn], func=AF.Identity, scale=inv), reads=(pmb,), writes=(meanb,))
        S.op("dve", lambda e: e.tensor_tensor(out=tmp[:, :nn], in0=mean[:, :nn], in1=mean[:, :nn], op=ALU.mult), reads=(meanb,), writes=(tmpb,))
        S.op("dve", lambda e: e.scalar_tensor_tensor(out=tmp[:, :nn], in0=pq[:, :nn], scalar=inv, in1=tmp[:, :nn], op0=ALU.mult, op1=ALU.subtract),
             reads=(pqb, tmpb), writes=(tmpb,))
        S.op("dve", lambda e: e.tensor_scalar(out=tmp[:, :nn], in0=tmp[:, :nn], scalar1=LN_EPS, scalar2=None, op0=ALU.add), reads=(tmpb,), writes=(tmpb,))
        S.op("act", lambda e: e.activation(out=tmp[:, :nn], in_=tmp[:, :nn], func=AF.Sqrt), reads=(tmpb,), writes=(tmpb,))
        S.op("dve", lambda e: e.reciprocal(out=rstd[:, :nn], in_=tmp[:, :nn]), reads=(tmpb,), writes=(rstdb,))
        S.op("dve", lambda e: e.scalar_tensor_tensor(out=mean[:, :nn], in0=mean[:, :nn], scalar=-1.0, in1=rstd[:, :nn], op0=ALU.mult, op1=ALU.mult),
             reads=(meanb, rstdb), writes=(meanb,))
        for ft in range(FT):
            xs = x[:, ft, n0:n0 + nn]
            S.op("dve", lambda e, xs=xs: e.tensor_tensor(out=xs, in0=xs.bitcast(F32), in1=rstd[:, :nn], op=ALU.mult), reads=(xbs[ft], rstdb), writes=(xbs[ft],))
            S.op("pool", lambda e, xs=xs: e.tensor_tensor(out=xs, in0=xs.bitcast(F32), in1=mean[:, :nn], op=ALU.add), reads=(xbs[ft], meanb), writes=(xbs[ft],))
            S.op("act", lambda e, xs=xs, ft=ft: e.activation(out=xs, in_=xs.bitcast(F32), func=AF.Identity, scale=g_ap(ft), bias=b_ap(ft)),
                 reads=(xbs[ft], gb_buf), writes=(xbs[ft],))

    for n0 in range(0, N, 512):
        chunk(n0, min(512, N - n0))


PV_BIN, PV_HYW, PV_HYB, PV_CFW, PV_CFB, PV_CFG, PV_CFBE = 0, 96, 150, 168, 354, 360, 366
PV_BO, PV_L1G, PV_L1B, PV_L2G, PV_L2B, PV_HF = 372, 388, 404, 420, 436, 452
PV_L = 456
PV_G0 = DEPTH * PV_L
PV_N = PV_G0 + 32


def cols(v):
    v = np.asarray(v, np.float32).reshape(-1)
    n = (v.size + 127) // 128
    o = np.zeros((n * 128,), np.float32)
    o[:v.size] = v
    return o.reshape(n, 128).T


def pack_pv(inp, nl):
    pv = np.zeros((128, PV_N), np.float32)
    for l in range(nl):
        b = l * PV_L
        pv[:, b + PV_BIN:b + PV_BIN + 96] = cols(inp['b_in'][l])
        for j in range(3):
            pv[:, b + PV_HYW + j * 18:b + PV_HYW + (j + 1) * 18] = cols(inp['hy_conv_w'][l, j])
        pv[:, b + PV_HYB:b + PV_HYB + 18] = cols(inp['hy_conv_b'][l])
        for j in range(31):
            pv[:, b + PV_CFW + j * 6:b + PV_CFW + (j + 1) * 6] = cols(inp['cf_dw_w'][l, j])
        pv[:, b + PV_CFB:b + PV_CFB + 6] = cols(inp['cf_dw_b'][l])
        pv[:, b + PV_CFG:b + PV_CFG + 6] = cols(inp['cf_ln_g'][l])
        pv[:, b + PV_CFBE:b + PV_CFBE + 6] = cols(inp['cf_ln_b'][l])
        pv[:, b + PV_BO:b + PV_BO + 16] = cols(inp['b_o'][l])
        pv[:, b + PV_L1G:b + PV_L1G + 16] = cols(inp['ln1_g'][l])
        pv[:, b + PV_L1B:b + PV_L1B + 16] = cols(inp['ln1_b'][l])
        pv[:, b + PV_L2G:b + PV_L2G + 16] = cols(inp['ln2_g'][l])
        pv[:, b + PV_L2B:b + PV_L2B + 16] = cols(inp['ln2_b'][l])
        for j, k in enumerate(('hy_f_b1', 'hy_f_b2', 'hy_f_b3', 'hy_f_freq')):
            pv[:64, b + PV_HF + j] = inp[k][l]
    pv[:, PV_G0:PV_G0 + 16] = cols(inp['in_ln_g'])
    pv[:, PV_G0 + 16:PV_G0 + 32] = cols(inp['in_ln_b'])
    return pv


def round_f32r(a):
    u = np.ascontiguousarray(a, np.float32).view(np.uint32)
    u = (u + np.uint32(0x800)) & np.uint32(0xFFFFF000)
    return u.view(np.float32)


_CONSTS = None


def host_consts():
    global _CONSTS
    if _CONSTS is not None:
        return _CONSTS
    L = T
    N = 2 * L
    t = np.arange(L, dtype=np.float64)
    f = np.arange(L, dtype=np.float64) + 0.5
    ang = 2.0 * np.pi * np.outer(t, f) / N
    C = np.cos(ang)
    Sm = -np.sin(ang)
    cmat = round_f32r(C.astype(np.float32))
    smat = round_f32r(Sm.astype(np.float32))
    gmat = round_f32r(np.concatenate([C.T, Sm.T], 0).astype(np.float32) * np.float32(2.0 / N))
    f32 = np.float32
    tt = np.linspace(0.0, 1.0, L, dtype=f32)[:, None]
    bands = 16
    w = (2.0 * math.pi * np.arange(L, dtype=f32)[:, None] / L).astype(f32)
    fb = np.linspace(1e-4, bands - 1, bands, dtype=f32)[None, :]
    z = np.concatenate([tt, np.cos(fb * w), -np.sin(fb * w)], axis=-1).astype(f32)
    max_decay = math.log(1e-2) / 0.3
    min_decay = math.log(1e-2) / 1.5
    deltas = np.abs(np.linspace(min_decay, max_decay, 3072, dtype=f32))
    dec = np.exp(-tt * deltas).astype(f32)
    qc = np.arange(64)[None, :]
    kc = np.arange(64)[:, None]
    cs = np.clip(qc - 8, 0, 48)
    valid = ((kc >= cs) & (kc < cs + 16)).astype(f32)
    mfull = np.zeros((64, 23, 64), f32)
    mint = np.zeros((64, 23, 64), f32)
    for i in range(15):
        mfull[:, i + 4, :] = valid
        if 4 <= i <= 11:
            mint[:, i + 4, :] = valid
    mfull, mint = pair_tab(mfull), pair_tab(mint)
    def tile_lhs(m, kt):
        return np.ascontiguousarray(m.reshape(kt, 128, m.shape[1] // 128, 128).transpose(2, 1, 0, 3)).reshape(m.shape[1] // 128, 128, kt * 128)
    gmat = tile_lhs(gmat, 32)
    _CONSTS = dict(cmat=cmat, smat=smat, gmat=gmat, cmatT=tile_lhs(cmat, 16), smatT=tile_lhs(smat, 16), zT=np.ascontiguousarray(z.T), dec=dec,
                   mfull=mfull, mint=mint, ident=np.concatenate([np.eye(128, dtype=f32), np.ones((128, 128), f32)], 1))
    return _CONSTS


def pair_tab(a):
    out = np.zeros(a.shape[:-3] + (128, 23, 64), np.float32)
    out[..., :64, :, :] = a
    out[..., 64:, 1:, :] = a[..., :, :-1, :]
    return out


def expand_rpb(rpb):
    nl = rpb.shape[0]
    qc = np.arange(64)[None, :]
    kc = np.arange(64)[:, None]
    co = np.clip(kc - qc, -15, 15) + 15
    out = np.zeros((nl, 12, 64, 23, 64), np.float32)
    for i in range(15):
        ro = 14 - i
        out[:, :, :, i + 4, :] = rpb[:, :, ro][:, :, co]
    return pair_tab(out)


class Prog:
    def __init__(self, nl, dbg=(), skip=(), ext_in=()):
        self.nl = nl
        nc = self.nc = bass.Bass("TRN2", target_bir_lowering=False)
        nc.dge_precook = False
        self.S = Sched(nc)
        self.dr = {}
        self.db = {}
        self.dbg = dbg

        def inp(name, shape, dt=F32R):
            if name in skip:
                return
            self.dr[name] = nc.dram_tensor(name, list(shape), dt, kind="ExternalInput").ap()
            self.db[name] = Buf(name)

        def scr(name, shape, dt=F32R):
            kind = "ExternalOutput" if name in dbg else ("ExternalInput" if name in ext_in else "Internal")
            self.dr[name] = nc.dram_tensor(name, list(shape), dt, kind=kind).ap()
            self.db[name] = Buf(name)
        inp("xT", [D, T])
        inp("pv", [128, PV_N], F32)
        inp("w_in", [nl, D, CIN])
        inp("rpbT", [nl, 12, 128, 23 * 64], F32)
        inp("hy_f_w1", [nl, 33, 64], F32)
        inp("hy_f_w2", [nl, 64, 64], F32)
        inp("hy_f_w3", [nl, 64, 64], F32)
        inp("hy_f_w4", [nl, 64, 3072], F32)
        inp("hy_skip", [nl, 2, 768], F32)
        inp("w_attn_br", [nl, 768, D])
        inp("w_hy_br", [nl, 768, D])
        inp("w_cf_br", [nl, 768, D])
        inp("w_o", [nl, D, D])
        inp("w_router", [D, 16])
        inp("b_router", [1, 16], F32)
        inp("moe_w_gate", [nl, NEXP, 8, 128, 16 * 128])
        inp("moe_w_up", [nl, NEXP, 8, 128, 16 * 128])
        inp("moe_w_down", [nl, NEXP, 2, 8, 128, 4 * 256])
        inp("cmat", [T, T])
        inp("smat", [T, T])
        inp("gmat", [16, 128, 32 * 128])
        inp("cmatT", [16, 128, 16 * 128])
        inp("smatT", [16, 128, 16 * 128])
        inp("zT", [33, T], F32)
        inp("dec", [T, 3072], F32)
        inp("mfull", [128, 23 * 64], F32)
        inp("mint", [128, 23 * 64], F32)
        inp("ident", [128, 256])
        scr("hTa", [D, T])
        scr("hTb", [D, T])
        scr("pT", [CIN, T])
        scr("yaT", [768, T])
        scr("yhT", [768, T])
        scr("ycT", [768, T])
        scr("mT", [D, T])
        scr("utok", [3, T, 768])
        scr("hsd", [4, T, 768])
        scr("Hsp", [4, T, 768], F32)
        scr("Ysp", [2 * T, 768])
        scr("z2", [T, 768])
        scr("yhtok", [T, 768])
        scr("cwT", [16, T], F32)
        self.dr["outT"] = nc.dram_tensor("outT", [D, T], F32R, kind="ExternalOutput").ap()
        self.db["outT"] = Buf("outT")
        self.pv = nc.alloc_sbuf_tensor("pv_sb", [128, PV_N], F32)
        self.pvb = Buf()
        self.io = nc.alloc_sbuf_tensor("ident_sb", [128, 256], F32R)
        self.identb = Buf()
        self.onesb = self.identb
        self.ident = self.io[:, 0:128]
        self.ones = self.io[:, 128:256]
        S = self.S
        S.load(self.pv[:], self.dr["pv"], self.pvb, self.db["pv"])
        S.load(self.io[:], self.dr["ident"], self.identb, self.db["ident"])
        S.barrier()
        S.emit()

    def pvc(self, col, n=1, parts=128):
        return self.pv[0:parts, col:col + n]

    def ln_block(self, src, dst, gcol, bcol, pre=None):
        S = self.S
        for tb in range(2):
            with Phase(S) as P:
                x, xb0 = P.sb([128, 16, 1024], F32R)
                xb = [xb0] + [Buf() for _ in range(15)]
                psums = [P.ps(), P.ps()]
                S.dma(x[:], wview(self.dr[src], tb * 1024, tb * 1024 + 1024), xb0, reads=(self.db[src],), writes=tuple(xb), split=2)
                ln_feature_major(S, P, x, xb, 16, 1024, lambda ft: self.pvc(gcol + ft), lambda ft: self.pvc(bcol + ft),
                                 self.pvb, self.ones, self.onesb, D, psums)
                S.dma(wview(self.dr[dst], tb * 1024, tb * 1024 + 1024), x[:], xb0, reads=tuple(xb), accw=(self.db[dst],), split=2, q="pool")

    def in_proj(self, l, hsrc):
        S = self.S
        w = self.dr["w_in"][l]
        pb = l * PV_L
        for tb in range(2):
            with Phase(S) as P:
                x, xb = P.sb([128, 16, 1024], F32R)
                wt = [P.sb([128, 16, 512], F32R) for _ in range(2)]
                ot = [P.sb([128, 512], F32R) for _ in range(3)]
                pss = [P.ps() for _ in range(4)]
                S.load(x[:], wview(self.dr[hsrc], tb * 1024, tb * 1024 + 1024), xb, self.db[hsrc])
                S.load(wt[0][0][:], wview(w, 0, 512), wt[0][1], self.db["w_in"])
                cnt = 0
                for mc in range(24):
                    wc, wcb = wt[mc % 2]
                    if mc + 1 < 24:
                        S.load(wt[(mc + 1) % 2][0][:], wview(w, (mc + 1) * 512, (mc + 2) * 512), wt[(mc + 1) % 2][1], self.db["w_in"])
                    for mi in range(4):
                        mt = mc * 4 + mi
                        for th in range(2):
                            ps, psb = pss[cnt % 4]
                            o, ob = ot[cnt % 3]
                            cnt += 1
                            mm_group(S, ps[:], psb, lambda kt, wc=wc, mi=mi: wc[:, kt, mi * 128:(mi + 1) * 128],
                                     lambda kt, th=th: x[:, kt, th * 512:(th + 1) * 512], 16, (wcb, xb))
                            S.op("act", lambda e, o=o, ps=ps, mt=mt: e.activation(out=o[:], in_=ps[:], func=AF.Identity,
                                                                                 bias=self.pvc(pb + PV_BIN + mt), scale=1.0),
                                 reads=(psb, self.pvb), writes=(ob,))
                            t0 = tb * 1024 + th * 512
                            S.store(self.dr["pT"][mt * 128:(mt + 1) * 128, t0:t0 + 512], o[:], ob, self.db["pT"])

    def attention(self, l):
        S = self.S
        pT, pTb = self.dr["pT"], self.db["pT"]
        groups = [(0, 4, list(range(0, 8, 2)), True)]
        for r0 in (4, 12, 20):
            groups.append((r0, r0 + 8, list(range(r0 - 4, r0 + 12, 2)), False))
        groups.append((28, 32, list(range(24, 32, 2)), True))
        with Phase(S) as P:
            mf, mfb = P.sb([128, 23 * 64], F32)
            mi, mib = P.sb([128, 23 * 64], F32)
            S.load(mf[:], self.dr["mfull"], mfb, self.db["mfull"], split=None)
            S.load(mi[:], self.dr["mint"], mib, self.db["mint"], split=None)
            hb = []
            for _ in range(2):
                hb.append(dict(q=P.sb([64, T], F32R), k=P.sb([64, T], F32R), vT=P.sb([64, T], F32R), v=P.sb([128, 16 * 64], F32R),
                               rp=P.sb([128, 23 * 64], F32), ef=P.sb([128, 23 * 64], F32), ei=P.sb([128, 23 * 64], F32)))
            es = [P.sb([128, 512], F32) for _ in range(4)]
            pp = [P.sb([128, 512], F32R) for _ in range(4)]
            rd, rdb = P.sb([64, 512], F32)
            ot = [P.sb([64, 512], F32R) for _ in range(2)]
            pss = [P.ps() for _ in range(4)]
            pos = [P.ps() for _ in range(2)]
            pds = [P.ps() for _ in range(2)]
            ident, ones = self.ident, self.ones
            cnt = [0, 0]

            def prep(h):
                B = hb[h % 2]
                (q, qb), (k, kb), (vT, vTb), (v, vb), (rp, rpb_), (ef, efb), (ei, eib) = (B[n] for n in ("q", "k", "vT", "v", "rp", "ef", "ei"))
                S.load(q[:], pT[O_Q + h * 64:O_Q + (h + 1) * 64, :], qb, pTb, split=None)
                S.load(k[:], pT[O_K + h * 64:O_K + (h + 1) * 64, :], kb, pTb, split=None)
                S.load(vT[:], pT[O_V + h * 64:O_V + (h + 1) * 64, :], vTb, pTb, split=None)
                S.load(rp[:], self.dr["rpbT"][l, h], rpb_, self.db["rpbT"], split=None)
                S.op("act", lambda e: e.activation(out=rp[:], in_=rp[:], func=AF.Exp), reads=(rpb_,), writes=(rpb_,))
                S.op("dve", lambda e: e.tensor_tensor(out=ef[:], in0=rp[:], in1=mf[:], op=ALU.mult), reads=(rpb_, mfb), writes=(efb,))
                S.op("pool", lambda e: e.tensor_tensor(out=ei[:], in0=rp[:], in1=mi[:], op=ALU.mult), reads=(rpb_, mib), writes=(eib,))

            def vtrans(h):
                B = hb[h % 2]
                (vT, vTb), (v, vb) = B["vT"], B["v"]
                for bnk in range(2):
                    ps, psb = pss[bnk]

                    def tr(e, ps=ps, bnk=bnk):
                        ins = None
                        for j in range(8):
                            pr = bnk * 8 + j
                            ins = e.matmul(ps[:, j * 64:(j + 1) * 64], lhsT=vT[:, pr * 128:(pr + 1) * 128], rhs=ident[0:64, 0:64], start=True, stop=True)
                        return ins
                    S.op("pe", tr, reads=(vTb, self.identb), writes=(psb,))
                    S.op("act", lambda e, ps=ps, bnk=bnk: e.activation(out=v[:, bnk * 512:(bnk + 1) * 512], in_=ps[:], func=AF.Identity),
                         reads=(psb,), writes=() if bnk else (vb,), accw=(vb,) if bnk else ())

            def front(h, st):
                B = hb[h % 2]
                (q, qb), (k, kb) = B["q"], B["k"]
                gi, r0, nq, kr, i, nk, full = st
                tab, tabb = B["ef"] if full else B["ei"]
                c = cnt[0]
                cnt[0] += 1
                ps, psb = pss[c % 4]
                e_s, e_sb = es[c % 4]
                p_, p_b = pp[c % 4]
                i0 = (r0 - kr + 7) + 4
                S.op("pe", lambda e: e.matmul(ps[:, :nq], lhsT=k[:, kr * 64:kr * 64 + 128], rhs=q[:, r0 * 64:r0 * 64 + nq], start=True, stop=True),
                     reads=(kb, qb), writes=(psb,))
                S.op("act", lambda e: e.activation(out=e_s[:, :nq], in_=ps[:, :nq], func=AF.Exp, scale=0.125), reads=(psb,), writes=(e_sb,))
                S.op("dve", lambda e: e.tensor_tensor(out=p_[:, :nq], in0=e_s[:, :nq], in1=tab[:, i0 * 64:i0 * 64 + nq], op=ALU.mult),
                     reads=(e_sb, tabb), writes=(p_b,))
                return (p_, p_b)

            def back(h, st, pbuf):
                B = hb[h % 2]
                (v, vb) = B["v"]
                gi, r0, nq, kr, i, nk, full = st
                p_, p_b = pbuf
                po, pob = pos[gi % 2]
                pd, pdb = pds[gi % 2]
                first, last = (i == 0), (i == nk - 1)
                pr = kr // 2
                S.op("pe", lambda e: e.matmul(po[0:64, :nq], lhsT=v[:, pr * 64:(pr + 1) * 64], rhs=p_[:, :nq], start=first, stop=last),
                     reads=(p_b, vb), writes=(pob,) if first else (), accw=() if first else (pob,))
                S.op("pe", lambda e: e.matmul(pd[0:64, :nq], lhsT=ones[:, 0:64], rhs=p_[:, :nq], start=first, stop=last),
                     reads=(p_b, self.onesb), writes=(pdb,) if first else (), accw=() if first else (pdb,))
                if last:
                    o, ob = ot[cnt[1] % 2]
                    cnt[1] += 1
                    S.op("act", lambda e: e.activation(out=rd[:, :nq], in_=pd[0:64, :nq], func=AF.Ln), reads=(pdb,), writes=(rdb,))
                    S.op("act", lambda e: e.activation(out=rd[:, :nq], in_=rd[:, :nq], func=AF.Exp, scale=-1.0), reads=(rdb,), writes=(rdb,))
                    S.op("dve", lambda e: e.tensor_tensor(out=o[:, :nq], in0=po[0:64, :nq], in1=rd[:, :nq], op=ALU.mult), reads=(pob, rdb), writes=(ob,))
                    S.store(self.dr["yaT"][h * 64:(h + 1) * 64, r0 * 64:r0 * 64 + nq], o[:, :nq], ob, self.db["yaT"], split=None)

            def head(h):
                steps = []
                for gi, (r0, r1, krs, full) in enumerate(groups):
                    for i, kr in enumerate(krs):
                        steps.append((gi + 5 * h, r0, (r1 - r0) * 64, kr, i, len(krs), full))
                pend = {}
                LA = 2
                for s_ in range(min(LA, len(steps))):
                    pend[s_] = front(h, steps[s_])
                for s_ in range(len(steps)):
                    if s_ + LA < len(steps):
                        pend[s_ + LA] = front(h, steps[s_ + LA])
                    back(h, steps[s_], pend.pop(s_))

            prep(0)
            for h in range(12):
                vtrans(h)
                if h + 1 < 12:
                    prep(h + 1)
                head(h)

    def conformer(self, l):
        S = self.S
        pT, pTb = self.dr["pT"], self.db["pT"]
        pb = l * PV_L
        with Phase(S) as P:
            zc, zcb0 = P.sb([128, 6, T], F32R)
            zcbs = [zcb0] + [Buf() for _ in range(5)]
            a, ab = P.sb([128, T], F32R)
            g, gb = P.sb([128, T], F32R)
            zps = [P.sb([128, T + 30], F32R) for _ in range(2)]
            dgs = [P.sb([128, 31, 128], F32R) for _ in range(2)]
            psums = [P.ps(), P.ps()]
            cps = [P.ps() for _ in range(4)]
            for zp, zpb in zps:
                S.op("dve", lambda e, zp=zp: e.tensor_scalar(out=zp[:, 0:15], in0=self.ident[:, 0:15].bitcast(F32), scalar1=0.0, scalar2=None, op0=ALU.mult),
                     reads=(self.identb,), accw=(zpb,))
                S.op("dve", lambda e, zp=zp: e.tensor_scalar(out=zp[:, 15 + T:30 + T], in0=self.ident[:, 0:15].bitcast(F32), scalar1=0.0, scalar2=None, op0=ALU.mult),
                     reads=(self.identb,), accw=(zpb,))
            cnt = [0]

            def ctile(ct):
                zp, zpb = zps[ct % 2]
                dg, dgb = dgs[ct % 2]
                zcb = zcbs[ct]
                S.load(a[:], pT[O_CF + ct * 128:O_CF + (ct + 1) * 128, :], ab, pTb, split=None)
                S.load(g[:], pT[O_CF + 768 + ct * 128:O_CF + 768 + (ct + 1) * 128, :], gb, pTb, split=None)
                for j in range(31):
                    S.op("pool", lambda e, j=j: e.tensor_scalar(out=dg[:, j, :], in0=self.ident.bitcast(F32), scalar1=self.pvc(pb + PV_CFW + j * 6 + ct), scalar2=None,
                                                                op0=ALU.mult),
                         reads=(self.identb, self.pvb), writes=(dgb,) if j == 0 else (), accw=() if j == 0 else (dgb,))
                S.op("act", lambda e: e.activation(out=g[:].bitcast(F32), in_=g[:].bitcast(F32), func=AF.Sigmoid), reads=(gb,), writes=(gb,))
                S.op("dve", lambda e: e.tensor_tensor(out=zp[:, 15:15 + T], in0=a[:].bitcast(F32), in1=g[:].bitcast(F32), op=ALU.mult),
                     reads=(ab, gb), accw=(zpb,))
                for tq in range(4):
                    ps, psb = cps[cnt[0] % 4]
                    cnt[0] += 1
                    mm_group(S, ps[:], psb, lambda j: dg[:, j, :], lambda j, tq=tq: zp[:, tq * 512 + j:tq * 512 + j + 512], 31, (dgb, zpb))
                    S.op("act", lambda e, ps=ps, tq=tq: e.activation(out=zc[:, ct, tq * 512:(tq + 1) * 512], in_=ps[:], func=AF.Identity,
                                                                     bias=self.pvc(pb + PV_CFB + ct), scale=1.0),
                         reads=(psb, self.pvb), writes=(zcb,) if tq == 0 else (), accw=() if tq == 0 else (zcb,))
            for ct in range(6):
                ctile(ct)
            ln_feature_major(S, P, zc, zcbs, 6, T, lambda ft: self.pvc(pb + PV_CFG + ft), lambda ft: self.pvc(pb + PV_CFBE + ft),
                             self.pvb, self.ones, self.onesb, 768, psums)
            for ct in range(6):
                S.op("act", lambda e, ct=ct: e.activation(out=zc[:, ct, :], in_=zc[:, ct, :].bitcast(F32), func=AF.Silu), reads=(zcbs[ct],), writes=(zcbs[ct],))
            S.dma(wview(self.dr["ycT"], 0, T), zc[:], zcb0, reads=tuple(zcbs), accw=(self.db["ycT"],), split=2, q="pool")

    def hyena(self, l):
        self.hy_conv3(l)
        self.hy_filter(l)
        self.hy_spectra()
        ut = self.dr["utok"]
        self.hy_fwd(0, ut[0], "utok")
        self.hy_inv(l, 0, ut[0], "utok", ut[1], self.dr["z2"], "z2")
        self.hy_fwd(1, self.dr["z2"], "z2")
        self.hy_inv(l, 1, self.dr["z2"], "z2", ut[2], self.dr["yhtok"], "yhtok")
        self.hy_out()

    def hy_conv3(self, l):
        S = self.S
        pT, pTb = self.dr["pT"], self.db["pT"]
        pb = l * PV_L
        with Phase(S) as P:
            us = [P.sb([128, T], F32R) for _ in range(2)]
            ucs = [P.sb([128, T], F32R) for _ in range(2)]
            ots = [P.sb([128, 4, 128], F32R) for _ in range(2)]
            pss = [P.ps() for _ in range(2)]
            cnt = [0]

            def ctile(ct):
                u, ub = us[ct % 2]
                uc, ucb = ucs[ct % 2]
                S.load(u[:], pT[O_HY + ct * 128:O_HY + (ct + 1) * 128, :], ub, pTb, split=None)
                S.op("act", lambda e: e.activation(out=uc[:], in_=u[:].bitcast(F32), func=AF.Identity, scale=self.pvc(pb + PV_HYW + 18 + ct),
                                                   bias=self.pvc(pb + PV_HYB + ct)), reads=(ub, self.pvb), writes=(ucb,))
                S.op("dve", lambda e: e.scalar_tensor_tensor(out=uc[:, 1:T], in0=u[:, 0:T - 1].bitcast(F32), scalar=self.pvc(pb + PV_HYW + ct),
                                                             in1=uc[:, 1:T].bitcast(F32), op0=ALU.mult, op1=ALU.add),
                     reads=(ub, ucb, self.pvb), writes=(ucb,))
                S.op("dve", lambda e: e.scalar_tensor_tensor(out=uc[:, 0:T - 1], in0=u[:, 1:T].bitcast(F32), scalar=self.pvc(pb + PV_HYW + 36 + ct),
                                                             in1=uc[:, 0:T - 1].bitcast(F32), op0=ALU.mult, op1=ALU.add),
                     reads=(ub, ucb, self.pvb), writes=(ucb,))
                s, c0 = ct // 6, (ct % 6) * 128
                dst = self.dr["utok"][s].rearrange("(tt p) c -> p tt c", p=128)
                for tq in range(4):
                    ps, psb = pss[cnt[0] % 2]
                    o, ob = ots[cnt[0] % 2]
                    cnt[0] += 1

                    def tr(e, ps=ps, tq=tq):
                        ins = None
                        for j in range(4):
                            tt = tq * 4 + j
                            ins = e.matmul(ps[:, j * 128:(j + 1) * 128], lhsT=uc[:, tt * 128:(tt + 1) * 128], rhs=self.ident, start=True, stop=True)
                        return ins
                    S.op("pe", tr, reads=(ucb, self.identb), writes=(psb,))
                    S.op("act", lambda e, ps=ps, o=o: e.activation(out=o[:].rearrange("p a b -> p (a b)"), in_=ps[:], func=AF.Identity),
                         reads=(psb,), writes=(ob,))
                    S.store(dst[:, tq * 4:(tq + 1) * 4, c0:c0 + 128], o[:], ob, self.db["utok"], split=None)
            for ct in range(18):
                ctile(ct)

    def hy_filter(self, l):
        S = self.S
        pb = l * PV_L + PV_HF
        with Phase(S) as P:
            zT, zTb = P.sb([33, T], F32)
            w1, w1b = P.sb([33, 64], F32)
            w2, w2b = P.sb([64, 64], F32)
            w3, w3b = P.sb([64, 64], F32)
            w4, w4b = P.sb([64, 3072], F32)
            hs_ = [P.sb([64, T], F32) for _ in range(2)]
            frb, frbb = P.sb([64, 4], F32)
            tt_, ttb = P.sb([64, 512], F32)
            mm_, mmb = P.sb([64, 512], F32)
            d0s = [P.sb([128, 384], F32) for _ in range(2)]
            d1s = [P.sb([128, 384], F32) for _ in range(2)]
            a0, a0b = P.sb([128, 384], F32)
            a1, a1b = P.sb([128, 384], F32)
            hss = [P.sb([128, 384], F32R) for _ in range(2)]
            hds = [P.sb([128, 384], F32R) for _ in range(2)]
            pss = [P.ps() for _ in range(4)]
            S.load(zT[:], self.dr["zT"], zTb, self.db["zT"], split=None)
            S.load(w1[:], self.dr["hy_f_w1"][l], w1b, self.db["hy_f_w1"], split=None)
            S.load(w2[:], self.dr["hy_f_w2"][l], w2b, self.db["hy_f_w2"], split=None)
            S.load(w3[:], self.dr["hy_f_w3"][l], w3b, self.db["hy_f_w3"], split=None)
            S.load(w4[:], self.dr["hy_f_w4"][l], w4b, self.db["hy_f_w4"], split=None)
            fr = self.pvc(pb + 3, 1, 64)
            for k in range(3):
                S.op("dve", lambda e, k=k: e.tensor_tensor(out=frb[:, k:k + 1], in0=self.pvc(pb + k, 1, 64), in1=fr, op=ALU.mult),
                     reads=(self.pvb,), writes=(frbb,))
            cur, curb, kd = zT, zTb, 33
            cnt = [0]
            for k, (w, wb) in enumerate(((w1, w1b), (w2, w2b), (w3, w3b))):
                nxt, nxtb = hs_[k % 2]

                def stage(c, cur=cur, curb=curb, kd=kd, w=w, wb=wb, nxt=nxt, nxtb=nxtb, k=k):
                    ps, psb = pss[cnt[0] % 4]
                    cnt[0] += 1
                    S.op("pe", lambda e: e.matmul(ps[0:64, :], lhsT=w[0:kd, :], rhs=cur[0:kd, c * 512:(c + 1) * 512], start=True, stop=True),
                         reads=(wb, curb), writes=(psb,))
                    S.op("dve", lambda e: e.tensor_scalar(out=tt_[:], in0=ps[0:64, :], scalar1=fr, scalar2=frb[:, k:k + 1], op0=ALU.mult, op1=ALU.add),
                         reads=(psb, frbb, self.pvb), writes=(ttb,))
                    S.op("dve", lambda e: e.tensor_scalar(out=mm_[:], in0=tt_[:], scalar1=PI, scalar2=-2.0 * PI, op0=ALU.is_gt, op1=ALU.mult),
                         reads=(ttb,), writes=(mmb,))
                    S.op("dve", lambda e: e.tensor_tensor(out=tt_[:], in0=tt_[:], in1=mm_[:], op=ALU.add), reads=(ttb, mmb), writes=(ttb,))
                    S.op("dve", lambda e: e.tensor_scalar(out=mm_[:], in0=tt_[:], scalar1=-PI, scalar2=2.0 * PI, op0=ALU.is_lt, op1=ALU.mult),
                         reads=(ttb,), writes=(mmb,))
                    S.op("dve", lambda e: e.tensor_tensor(out=tt_[:], in0=tt_[:], in1=mm_[:], op=ALU.add), reads=(ttb, mmb), writes=(ttb,))
                    S.op("act", lambda e: e.activation(out=nxt[:, c * 512:(c + 1) * 512], in_=tt_[:], func=AF.Sin), reads=(ttb,), writes=(nxtb,))
                for c in range(4):
                    stage(c)
                cur, curb, kd = nxt, nxtb, 64
            h3, h3b = cur, curb
            it = [0]

            def taps(pt, o, c2):
                i = it[0]
                it[0] += 1
                ps0, ps0b = pss[(2 * i) % 4]
                ps1, ps1b = pss[(2 * i + 1) % 4]
                d0, d0b = d0s[i % 2]
                d1, d1b = d1s[i % 2]
                hs, hsb = hss[i % 2]
                hd, hdb = hds[i % 2]
                cA = o * 768 + c2 * 384
                cB = 1536 + cA
                S.load(d0[:], self.dr["dec"][pt * 128:(pt + 1) * 128, cA:cA + 384], d0b, self.db["dec"], split=None)
                S.load(d1[:], self.dr["dec"][pt * 128:(pt + 1) * 128, cB:cB + 384], d1b, self.db["dec"], split=None)
                S.op("pe", lambda e: e.matmul(ps0[:, 0:384], lhsT=h3[:, pt * 128:(pt + 1) * 128], rhs=w4[:, cA:cA + 384], start=True, stop=True),
                     reads=(h3b, w4b), writes=(ps0b,))
                S.op("pe", lambda e: e.matmul(ps1[:, 0:384], lhsT=h3[:, pt * 128:(pt + 1) * 128], rhs=w4[:, cB:cB + 384], start=True, stop=True),
                     reads=(h3b, w4b), writes=(ps1b,))
                S.op("dve", lambda e: e.tensor_tensor(out=a0[:], in0=ps0[:, 0:384], in1=d0[:], op=ALU.mult), reads=(ps0b, d0b), writes=(a0b,))
                S.op("dve", lambda e: e.tensor_tensor(out=a1[:], in0=ps1[:, 0:384], in1=d1[:], op=ALU.mult), reads=(ps1b, d1b), writes=(a1b,))
                if pt == 0:
                    S.op("dve", lambda e: e.memset(a1[0:1, :], 0.0), reads=(a1b,), writes=(a1b,))
                S.op("dve", lambda e: e.tensor_tensor(out=hs[:], in0=a0[:], in1=a1[:], op=ALU.add), reads=(a0b, a1b), writes=(hsb,))
                S.op("dve", lambda e: e.tensor_tensor(out=hd[:], in0=a0[:], in1=a1[:], op=ALU.subtract), reads=(a0b, a1b), writes=(hdb,))
                S.store(self.dr["hsd"][o][pt * 128:(pt + 1) * 128, c2 * 384:(c2 + 1) * 384], hs[:], hsb, self.db["hsd"], split=None)
                S.store(self.dr["hsd"][2 + o][pt * 128:(pt + 1) * 128, c2 * 384:(c2 + 1) * 384], hd[:], hdb, self.db["hsd"], split=None)
            for pt in range(16):
                for o in range(2):
                    for c2 in range(2):
                        taps(pt, o, c2)

    def hy_spectra(self):
        S = self.S
        for mi_, mat in enumerate(("cmat", "smat")):
            with Phase(S) as P:
                xs = [P.sb([128, 16, 768], F32R) for _ in range(2)]
                wt = [P.sb([128, 16, 512], F32R) for _ in range(2)]
                ots = [P.sb([128, 768], F32) for _ in range(2)]
                pss = [P.ps() for _ in range(4)]
                for o in range(2):
                    S.load(xs[o][0][:], self.dr["hsd"][2 * mi_ + o].rearrange("(kt p) c -> p kt c", p=128), xs[o][1], self.db["hsd"])
                w = self.dr[mat]
                S.load(wt[0][0][:], wview(w, 0, 512), wt[0][1], self.db[mat])
                cnt = [0, 0]
                for mc in range(4):
                    wc, wcb = wt[mc % 2]
                    if mc + 1 < 4:
                        S.load(wt[(mc + 1) % 2][0][:], wview(w, (mc + 1) * 512, (mc + 2) * 512), wt[(mc + 1) % 2][1], self.db[mat])
                    for mi in range(4):
                        ft = mc * 4 + mi
                        for o in range(2):
                            x, xb = xs[o]
                            ot, otb = ots[cnt[1] % 2]
                            cnt[1] += 1
                            for (n0, nn) in ((0, 512), (512, 256)):
                                ps, psb = pss[cnt[0] % 4]
                                cnt[0] += 1
                                mm_group(S, ps[:, :nn], psb, lambda kt, wc=wc, mi=mi: wc[:, kt, mi * 128:(mi + 1) * 128],
                                         lambda kt, x=x, n0=n0, nn=nn: x[:, kt, n0:n0 + nn], 16, (wcb, xb))
                                S.op("act", lambda e, ot=ot, ps=ps, n0=n0, nn=nn: e.activation(out=ot[:, n0:n0 + nn], in_=ps[:, :nn], func=AF.Identity),
                                     reads=(psb,), writes=() if n0 else (otb,), accw=(otb,) if n0 else ())
                            S.store(self.dr["Hsp"][2 * mi_ + o][ft * 128:(ft + 1) * 128, :], ot[:], otb, self.db["Hsp"], split=None)

    def hy_fwd(self, o, zsrc, zname):
        S = self.S
        with Phase(S) as P:
            x, xb = P.sb([128, 16, 768], F32R)
            cws = [P.sb([128, 16, 128], F32R) for _ in range(2)]
            sws = [P.sb([128, 16, 128], F32R) for _ in range(2)]
            hrs = [P.sb([128, 768], F32) for _ in range(2)]
            his = [P.sb([128, 768], F32) for _ in range(2)]
            t1, t1b = P.sb([128, 384], F32)
            t2, t2b = P.sb([128, 384], F32)
            yrs = [P.sb([128, 768], F32R) for _ in range(2)]
            yis = [P.sb([128, 768], F32R) for _ in range(2)]
            pss = [P.ps() for _ in range(8)]
            S.load(x[:], zsrc.rearrange("(kt p) c -> p kt c", p=128), xb, self.db[zname])

            def ftile(ft):
                cw, cwb = cws[ft % 2]
                sw, swb = sws[ft % 2]
                hr, hrb = hrs[ft % 2]
                hi, hib = his[ft % 2]
                yr, yrb = yrs[ft % 2]
                yi, yib = yis[ft % 2]
                S.load(cw[:].rearrange("p a b -> p (a b)"), self.dr["cmatT"][ft], cwb, self.db["cmatT"], split=None)
                S.load(sw[:].rearrange("p a b -> p (a b)"), self.dr["smatT"][ft], swb, self.db["smatT"], split=None)
                S.load(hr[:], self.dr["Hsp"][o][ft * 128:(ft + 1) * 128, :], hrb, self.db["Hsp"], split=None)
                S.load(hi[:], self.dr["Hsp"][2 + o][ft * 128:(ft + 1) * 128, :], hib, self.db["Hsp"], split=None)
                for hf in range(2):
                    n0 = hf * 384
                    pre, preb = pss[(ft % 2) * 4 + hf * 2]
                    pim, pimb = pss[(ft % 2) * 4 + hf * 2 + 1]
                    mm_group(S, pre[:, :384], preb, lambda kt: cw[:, kt, :], lambda kt, n0=n0: x[:, kt, n0:n0 + 384], 16, (cwb, xb))
                    mm_group(S, pim[:, :384], pimb, lambda kt: sw[:, kt, :], lambda kt, n0=n0: x[:, kt, n0:n0 + 384], 16, (swb, xb))
                    sl = slice(n0, n0 + 384)
                    TT = lambda o_, a_, b_, op_: (lambda e: e.tensor_tensor(out=o_, in0=a_, in1=b_, op=op_))
                    S.op("dve", TT(t1[:], pre[:, :384], hr[:, sl], ALU.mult), reads=(preb, hrb), writes=(t1b,))
                    S.op("dve", TT(t2[:], pim[:, :384], hi[:, sl], ALU.mult), reads=(pimb, hib), writes=(t2b,))
                    S.op("dve", TT(yr[:, sl], t1[:], t2[:], ALU.subtract), reads=(t1b, t2b), writes=() if hf else (yrb,), accw=(yrb,) if hf else ())
                    S.op("dve", TT(t1[:], pre[:, :384], hi[:, sl], ALU.mult), reads=(preb, hib), writes=(t1b,))
                    S.op("dve", TT(t2[:], pim[:, :384], hr[:, sl], ALU.mult), reads=(pimb, hrb), writes=(t2b,))
                    S.op("dve", TT(yi[:, sl], t1[:], t2[:], ALU.add), reads=(t1b, t2b), writes=() if hf else (yib,), accw=(yib,) if hf else ())
                S.store(self.dr["Ysp"][ft * 128:(ft + 1) * 128, :], yr[:], yrb, self.db["Ysp"], split=None)
                S.store(self.dr["Ysp"][T + ft * 128:T + (ft + 1) * 128, :], yi[:], yib, self.db["Ysp"], split=None)
            for ft in range(16):
                ftile(ft)

    def hy_inv(self, l, o, zsrc, zname, xm, dst, dname):
        S = self.S
        with Phase(S) as P:
            y, yb = P.sb([128, 32, 768], F32R)
            wt = [P.sb([128, 32, 128], F32R) for _ in range(2)]
            sk, skb = P.sb([128, 768], F32)
            zs = [P.sb([128, 768], F32R) for _ in range(2)]
            xms = [P.sb([128, 768], F32R) for _ in range(2)]
            a, ab = P.sb([128, 768], F32)
            b2, b2b = P.sb([128, 384], F32)
            ots = [P.sb([128, 768], F32R) for _ in range(2)]
            pss = [P.ps() for _ in range(4)]
            S.load(y[:], self.dr["Ysp"].rearrange("(kt p) c -> p kt c", p=128), yb, self.db["Ysp"])
            S.load(sk[:], self.dr["hy_skip"][l, o].partition_broadcast(128), skb, self.db["hy_skip"], split=None)

            def ttile(tt):
                w, wb = wt[tt % 2]
                z, zb = zs[tt % 2]
                xmt, xmb = xms[tt % 2]
                ot, otb = ots[tt % 2]
                S.load(w[:].rearrange("p a b -> p (a b)"), self.dr["gmat"][tt], wb, self.db["gmat"], split=None)
                S.load(z[:], zsrc[tt * 128:(tt + 1) * 128, :], zb, self.db[zname], split=None)
                S.load(xmt[:], xm[tt * 128:(tt + 1) * 128, :], xmb, self.db["utok"], split=None)
                S.op("dve", lambda e: e.tensor_tensor(out=a[:], in0=z[:].bitcast(F32), in1=sk[:], op=ALU.mult), reads=(zb, skb), writes=(ab,))
                for hf in range(2):
                    n0 = hf * 384
                    ps, psb = pss[(tt % 2) * 2 + hf]
                    mm_group(S, ps[:, :384], psb, lambda kt: w[:, kt, :], lambda kt, n0=n0: y[:, kt, n0:n0 + 384], 32, (wb, yb))
                    S.op("dve", lambda e, ps=ps, n0=n0: e.tensor_tensor(out=b2[:], in0=ps[:, :384], in1=a[:, n0:n0 + 384], op=ALU.add),
                         reads=(psb, ab), writes=(b2b,))
                    S.op("dve", lambda e, n0=n0: e.tensor_tensor(out=ot[:, n0:n0 + 384], in0=b2[:], in1=xmt[:, n0:n0 + 384].bitcast(F32), op=ALU.mult),
                         reads=(b2b, xmb), writes=() if hf else (otb,), accw=(otb,) if hf else ())
                S.store(dst[tt * 128:(tt + 1) * 128, :], ot[:], otb, self.db[dname], split=None)
            for tt in range(16):
                ttile(tt)

    def hy_out(self):
        S = self.S
        with Phase(S) as P:
            y, yb = P.sb([128, 16, 768], F32R)
            ots = [P.sb([128, 512], F32R) for _ in range(2)]
            pss = [P.ps() for _ in range(2)]
            S.load(y[:], self.dr["yhtok"].rearrange("(kt p) c -> p kt c", p=128), yb, self.db["yhtok"])
            cnt = [0]
            for ct in range(6):
                for tq in range(4):
                    ps, psb = pss[cnt[0] % 2]
                    o, ob = ots[cnt[0] % 2]
                    cnt[0] += 1

                    def tr(e, ps=ps, tq=tq, ct=ct):
                        ins = None
                        for j in range(4):
                            ins = e.matmul(ps[:, j * 128:(j + 1) * 128], lhsT=y[:, tq * 4 + j, ct * 128:(ct + 1) * 128], rhs=self.ident, start=True, stop=True)
                        return ins
                    S.op("pe", tr, reads=(yb, self.identb), writes=(psb,))
                    S.op("act", lambda e, ps=ps, o=o: e.activation(out=o[:], in_=ps[:], func=AF.Identity), reads=(psb,), writes=(ob,))
                    S.store(self.dr["yhT"][ct * 128:(ct + 1) * 128, tq * 512:(tq + 1) * 512], o[:], ob, self.db["yhT"], split=None)

    def merge(self, l):
        S = self.S
        pT, pTb = self.dr["pT"], self.db["pT"]
        brs = (("yaT", "w_attn_br", O_GA), ("yhT", "w_hy_br", O_GH), ("ycT", "w_cf_br", O_GC))
        for tb in range(2):
            with Phase(S) as P:
                ys = [P.sb([128, 6, 1024], F32R) for _ in range(3)]
                ws = [[P.sb([128, 6, 128], F32R) for _ in range(2)] for _ in range(3)]
                gts = [P.sb([128, 512], F32R) for _ in range(3)]
                sgs = [P.sb([128, 512], F32) for _ in range(2)]
                tmp, tmpb = P.sb([128, 512], F32)
                accs = [P.sb([128, 512], F32R) for _ in range(2)]
                pss = [P.ps() for _ in range(4)]
                for i, (yn, wn, og) in enumerate(brs):
                    S.load(ys[i][0][:], wview(self.dr[yn], tb * 1024, (tb + 1) * 1024), ys[i][1], self.db[yn])
                cnt = [0, 0]

                def mtile(mt):
                    for i, (yn, wn, og) in enumerate(brs):
                        w, wb = ws[i][mt % 2]
                        S.load(w[:], wview(self.dr[wn][l], mt * 128, (mt + 1) * 128), wb, self.db[wn], split=None)
                    for th in range(2):
                        acc, accb = accs[cnt[1] % 2]
                        cnt[1] += 1
                        t0 = tb * 1024 + th * 512
                        for i, (yn, wn, og) in enumerate(brs):
                            w, wb = ws[i][mt % 2]
                            y, yb = ys[i]
                            ps, psb = pss[cnt[0] % 4]
                            sg, sgb = sgs[cnt[0] % 2]
                            gt, gtb = gts[i]
                            cnt[0] += 1
                            mm_group(S, ps[:], psb, lambda kt, w=w: w[:, kt, :], lambda kt, y=y, th=th: y[:, kt, th * 512:(th + 1) * 512], 6, (wb, yb))
                            S.load(gt[:], pT[og + mt * 128:og + (mt + 1) * 128, t0:t0 + 512], gtb, pTb, split=None)
                            S.op("act", lambda e, sg=sg, gt=gt: e.activation(out=sg[:], in_=gt[:].bitcast(F32), func=AF.Sigmoid), reads=(gtb,), writes=(sgb,))
                            if i == 0:
                                S.op("dve", lambda e, acc=acc, ps=ps, sg=sg: e.tensor_tensor(out=acc[:], in0=ps[:], in1=sg[:], op=ALU.mult),
                                     reads=(psb, sgb), writes=(accb,))
                            else:
                                S.op("dve", lambda e, ps=ps, sg=sg: e.tensor_tensor(out=tmp[:], in0=ps[:], in1=sg[:], op=ALU.mult),
                                     reads=(psb, sgb), writes=(tmpb,))
                                S.op("dve", lambda e, acc=acc: e.tensor_tensor(out=acc[:], in0=acc[:].bitcast(F32), in1=tmp[:], op=ALU.add),
                                     reads=(accb, tmpb), writes=(accb,))
                        S.store(self.dr["mT"][mt * 128:(mt + 1) * 128, t0:t0 + 512], acc[:], accb, self.db["mT"], split=None)
                for mt in range(16):
                    mtile(mt)

    def wo_ln(self, l, hsrc, hdst):
        S = self.S
        pb = l * PV_L
        w = self.dr["w_o"][l]
        for tb in range(2):
            with Phase(S) as P:
                m, mb = P.sb([128, 16, 1024], F32R)
                xn, xnb0 = P.sb([128, 16, 1024], F32R)
                xnbs = [xnb0] + [Buf() for _ in range(15)]
                wt = [P.sb([128, 16, 256], F32R) for _ in range(2)]
                hts = [P.sb([128, 512], F32R) for _ in range(2)]
                pss = [P.ps() for _ in range(4)]
                lnps = [P.ps(), P.ps()]
                S.load(m[:], wview(self.dr["mT"], tb * 1024, (tb + 1) * 1024), mb, self.db["mT"])
                S.load(wt[0][0][:], wview(w, 0, 256), wt[0][1], self.db["w_o"])
                cnt = [0]
                for mc in range(8):
                    wc, wcb = wt[mc % 2]
                    if mc + 1 < 8:
                        S.load(wt[(mc + 1) % 2][0][:], wview(w, (mc + 1) * 256, (mc + 2) * 256), wt[(mc + 1) % 2][1], self.db["w_o"])
                    for mi in range(2):
                        mt = mc * 2 + mi
                        for th in range(2):
                            ps, psb = pss[cnt[0] % 4]
                            ht, htb = hts[cnt[0] % 2]
                            cnt[0] += 1
                            t0 = tb * 1024 + th * 512
                            xs = xn[:, mt, th * 512:(th + 1) * 512]
                            mm_group(S, ps[:], psb, lambda kt, wc=wc, mi=mi: wc[:, kt, mi * 128:(mi + 1) * 128],
                                     lambda kt, th=th: m[:, kt, th * 512:(th + 1) * 512], 16, (wcb, mb))
                            S.load(ht[:], self.dr[hsrc][mt * 128:(mt + 1) * 128, t0:t0 + 512], htb, self.db[hsrc], split=None)
                            S.op("act", lambda e, xs=xs, ps=ps, mt=mt: e.activation(out=xs, in_=ps[:], func=AF.Identity, bias=self.pvc(pb + PV_BO + mt), scale=1.0),
                                 reads=(psb, self.pvb), accw=(xnbs[mt],))
                            S.op("dve", lambda e, xs=xs, ht=ht: e.scalar_tensor_tensor(out=xs, in0=ht[:].bitcast(F32), scalar=ALPHA, in1=xs.bitcast(F32),
                                                                                      op0=ALU.mult, op1=ALU.add),
                                 reads=(htb, xnbs[mt]), accw=(xnbs[mt],))
                ln_feature_major(S, P, xn, xnbs, 16, 1024, lambda ft: self.pvc(pb + PV_L1G + ft), lambda ft: self.pvc(pb + PV_L1B + ft),
                                 self.pvb, self.ones, self.onesb, D, lnps)
                S.dma(wview(self.dr[hdst], tb * 1024, (tb + 1) * 1024), xn[:], xnb0, reads=tuple(xnbs), accw=(self.db[hdst],), split=2, q="pool")

    def router(self, hsrc):
        S = self.S
        with Phase(S) as P:
            wr, wrb = P.sb([128, 16, 16], F32R)
            br, brb = P.sb([128, 16], F32)
            xs = [P.sb([128, 16, 128], F32R) for _ in range(2)]
            pss = [P.ps() for _ in range(2)]
            pts = [P.ps() for _ in range(2)]
            cts = [P.sb([16, 128], F32) for _ in range(2)]
            S.load(wr[:], self.dr["w_router"].rearrange("(kt p) e -> p kt e", p=128), wrb, self.db["w_router"], split=None)
            S.load(br[:], self.dr["b_router"][0].partition_broadcast(128), brb, self.db["b_router"], split=None)

            def tile(tt):
                x, xb = xs[tt % 2]
                ps, psb = pss[tt % 2]
                pt, ptb = pts[tt % 2]
                ct, ctb = cts[tt % 2]
                S.load(x[:], wview(self.dr[hsrc], tt * 128, (tt + 1) * 128), xb, self.db[hsrc], split=8)
                mm_group(S, ps[:, 0:16], psb, lambda kt: x[:, kt, :], lambda kt: wr[:, kt, :], 16, (xb, wrb))
                names = ("lg", "ex", "em", "sel", "m1", "m2", "sc", "gs", "mx", "s1")
                shp = dict(lg=16, ex=16, em=16, sel=16, m1=4, m2=4, sc=4, gs=4, mx=1, s1=1)
                t = {n: P.sb([128, shp[n]], F32) for n in names}
                cw, cwb = P.sb([128, 16], F32R)

                def A(n):
                    return t[n][0]

                def B(n):
                    return t[n][1]
                g3 = lambda ap: ap.rearrange("p (g e) -> p g e", g=4)
                S.op("dve", lambda e: e.tensor_tensor(out=A("lg")[:], in0=ps[:, 0:16], in1=br[:], op=ALU.add), reads=(psb, brb), writes=(B("lg"),))
                S.op("dve", lambda e: e.tensor_reduce(out=A("mx")[:], in_=A("lg")[:], axis=mybir.AxisListType.X, op=ALU.max), reads=(B("lg"),), writes=(B("mx"),))
                S.op("dve", lambda e: e.tensor_scalar(out=A("mx")[:], in0=A("mx")[:], scalar1=-1.0, scalar2=None, op0=ALU.mult), reads=(B("mx"),), writes=(B("mx"),))
                S.op("act", lambda e: e.activation(out=A("ex")[:], in_=A("lg")[:], func=AF.Exp, bias=A("mx")[:, 0:1], scale=1.0),
                     reads=(B("lg"), B("mx")), writes=(B("ex"),))
                S.op("dve", lambda e: e.tensor_reduce(out=A("m1")[:], in_=g3(A("ex")[:]), axis=mybir.AxisListType.X, op=ALU.max), reads=(B("ex"),), writes=(B("m1"),))
                for g in range(4):
                    S.op("dve", lambda e, g=g: e.tensor_scalar(out=A("em")[:, 4 * g:4 * g + 4], in0=A("ex")[:, 4 * g:4 * g + 4], scalar1=A("m1")[:, g:g + 1],
                                                               scalar2=-1e30, op0=ALU.is_equal, op1=ALU.mult),
                         reads=(B("ex"), B("m1")), writes=() if g else (B("em"),), accw=(B("em"),) if g else ())
                S.op("dve", lambda e: e.tensor_tensor(out=A("em")[:], in0=A("em")[:], in1=A("ex")[:], op=ALU.add), reads=(B("em"), B("ex")), writes=(B("em"),))
                S.op("dve", lambda e: e.tensor_reduce(out=A("m2")[:], in_=g3(A("em")[:]), axis=mybir.AxisListType.X, op=ALU.max), reads=(B("em"),), writes=(B("m2"),))
                S.op("dve", lambda e: e.tensor_tensor(out=A("sc")[:], in0=A("m1")[:], in1=A("m2")[:], op=ALU.add), reads=(B("m1"), B("m2")), writes=(B("sc"),))
                S.op("dve", lambda e: e.tensor_reduce(out=A("s1")[:], in_=A("sc")[:], axis=mybir.AxisListType.X, op=ALU.max), reads=(B("sc"),), writes=(B("s1"),))
                S.op("dve", lambda e: e.tensor_scalar(out=A("gs")[:], in0=A("sc")[:], scalar1=A("s1")[:, 0:1], scalar2=None, op0=ALU.is_equal),
                     reads=(B("sc"), B("s1")), writes=(B("gs"),))
                for g in range(4):
                    S.op("dve", lambda e, g=g: e.tensor_scalar(out=A("sel")[:, 4 * g:4 * g + 4], in0=A("ex")[:, 4 * g:4 * g + 4], scalar1=A("m2")[:, g:g + 1],
                                                               scalar2=A("gs")[:, g:g + 1], op0=ALU.is_ge, op1=ALU.mult),
                         reads=(B("ex"), B("m2"), B("gs")), writes=() if g else (B("sel"),), accw=(B("sel"),) if g else ())
                S.op("dve", lambda e: e.tensor_tensor(out=A("sel")[:], in0=A("sel")[:], in1=A("ex")[:], op=ALU.mult), reads=(B("sel"), B("ex")), writes=(B("sel"),))
                S.op("dve", lambda e: e.tensor_reduce(out=A("s1")[:], in_=A("sel")[:], axis=mybir.AxisListType.X, op=ALU.add), reads=(B("sel"),), writes=(B("s1"),))
                S.op("dve", lambda e: e.reciprocal(out=A("s1")[:], in_=A("s1")[:]), reads=(B("s1"),), writes=(B("s1"),))
                S.op("dve", lambda e: e.tensor_scalar(out=cw[:], in0=A("sel")[:], scalar1=A("s1")[:, 0:1], scalar2=None, op0=ALU.mult),
                     reads=(B("sel"), B("s1")), writes=(cwb,))
                S.op("pe", lambda e: e.matmul(pt[0:16, 0:128], lhsT=cw[:, :], rhs=self.ident, start=True, stop=True), reads=(cwb, self.identb), writes=(ptb,))
                S.op("act", lambda e: e.activation(out=ct[:], in_=pt[0:16, 0:128], func=AF.Identity), reads=(ptb,), writes=(ctb,))
                S.store(self.dr["cwT"][:, tt * 128:(tt + 1) * 128], ct[:], ctb, self.db["cwT"], split=None)
            for tt in range(16):
                tile(tt)

    def moe(self, l, hsrc, hdst):
        S = self.S
        pb = l * PV_L
        self.router(hsrc)
        TB = 1024
        for tbk in range(T // TB):
            with Phase(S) as P:
                x, xb = P.sb([128, 16, TB], F32R)
                ya, yab0 = P.sb([128, 16, TB], F32R)
                yabs = [yab0] + [Buf() for _ in range(15)]
                hs, hsb = P.sb([128, 4, TB], F32R)
                wgs = [P.sb([128, 16, 128], F32R) for _ in range(2)]
                wus = [P.sb([128, 16, 128], F32R) for _ in range(2)]
                wds = [P.sb([128, 4, 256], F32R) for _ in range(2)]
                cwb_, cwbb = P.sb([128, TB], F32)
                scr = [P.sb([128, 512], F32R), P.sb([128, 512], F32), P.sb([128, 512], F32), P.sb([128, 512], F32)]
                (tg, tgb), (tg2, tg2b) = scr[1], scr[2]
                psg = [P.ps() for _ in range(2)]
                psu = [P.ps() for _ in range(2)]
                psy = [P.ps() for _ in range(2)]
                lnps = [P.ps(), P.ps()]
                t0 = tbk * TB
                S.load(x[:], wview(self.dr[hsrc], t0, t0 + TB), xb, self.db[hsrc])
                for mt in range(16):
                    S.op("act", lambda e, mt=mt: e.activation(out=ya[:, mt, :], in_=x[:, mt, :].bitcast(F32), func=AF.Identity, scale=ALPHA),
                         reads=(xb,), writes=(yabs[mt],))
                cnt = [0, 0, 0, 0]

                def expert(e_):
                    S.load(cwb_[:], self.dr["cwT"][e_, t0:t0 + TB].partition_broadcast(128), cwbb, self.db["cwT"], split=None)
                    wgd = self.dr["moe_w_gate"][l, e_]
                    wud = self.dr["moe_w_up"][l, e_]
                    wdd = self.dr["moe_w_down"][l, e_]
                    for fh in range(2):
                        for fi in range(4):
                            ft = fh * 4 + fi
                            i = cnt[0]
                            cnt[0] += 1
                            wg, wgb = wgs[i % 2]
                            wu, wub = wus[i % 2]
                            S.load(wg[:].rearrange("p a b -> p (a b)"), wgd[ft], wgb, self.db["moe_w_gate"], split=None)
                            S.load(wu[:].rearrange("p a b -> p (a b)"), wud[ft], wub, self.db["moe_w_up"], split=None)
                            for th in range(2):
                                k_ = cnt[3]
                                cnt[3] += 1
                                pg, pgb = psg[k_ % 2]
                                pu, pub = psu[k_ % 2]
                                ts = slice(th * 512, (th + 1) * 512)
                                mm_group(S, pg[:], pgb, lambda kt, wg=wg: wg[:, kt, :], lambda kt, ts=ts: x[:, kt, ts], 16, (wgb, xb))
                                mm_group(S, pu[:], pub, lambda kt, wu=wu: wu[:, kt, :], lambda kt, ts=ts: x[:, kt, ts], 16, (wub, xb))
                                S.op("act", lambda e, pg=pg: e.activation(out=tg[:], in_=pg[:], func=AF.Silu), reads=(pgb,), writes=(tgb,))
                                S.op("pool", lambda e, ts=ts: e.tensor_tensor(out=tg2[:], in0=tg[:], in1=cwb_[:, ts], op=ALU.mult), reads=(tgb, cwbb), writes=(tg2b,))
                                first = (fi == 0 and th == 0)
                                S.op("dve", lambda e, pu=pu, fi=fi, ts=ts: e.tensor_tensor(out=hs[:, fi, ts], in0=pu[:], in1=tg2[:], op=ALU.mult),
                                     reads=(pub, tg2b), writes=(hsb,) if first else (), accw=() if first else (hsb,))
                        for dc in range(8):
                            j = cnt[1]
                            cnt[1] += 1
                            wd, wdb = wds[j % 2]
                            S.load(wd[:].rearrange("p a b -> p (a b)"), wdd[fh, dc], wdb, self.db["moe_w_down"], split=None)
                            for mi in range(2):
                                mt = dc * 2 + mi
                                for th in range(2):
                                    ts = slice(th * 512, (th + 1) * 512)
                                    py, pyb = psy[cnt[2] % 2]
                                    cnt[2] += 1
                                    mm_group(S, py[:], pyb, lambda kt, wd=wd, mi=mi: wd[:, kt, mi * 128:(mi + 1) * 128], lambda kt, ts=ts: hs[:, kt, ts], 4, (wdb, hsb))
                                    S.op("dve", lambda e, py=py, mt=mt, ts=ts: e.tensor_tensor(out=ya[:, mt, ts], in0=py[:], in1=ya[:, mt, ts].bitcast(F32), op=ALU.add),
                                         reads=(pyb, yabs[mt]), accw=(yabs[mt],))
                for e_ in range(NEXP):
                    expert(e_)
                ln_feature_major(S, P, ya, yabs, 16, TB, lambda ft: self.pvc(pb + PV_L2G + ft), lambda ft: self.pvc(pb + PV_L2B + ft),
                                 self.pvb, self.ones, self.onesb, D, lnps, scratch=scr)
                S.dma(wview(self.dr[hdst], t0, t0 + TB), ya[:], yab0, reads=tuple(yabs), accw=(self.db[hdst],), split=2, q="pool")

    def layer(self, l, last):
        self.in_proj(l, "hTa")
        self.attention(l)
        self.hyena(l)
        self.conformer(l)
        self.merge(l)
        self.wo_ln(l, "hTa", "hTb")
        self.moe(l, "hTb", "outT" if last else "hTa")

    def full(self):
        self.ln_block("xT", "hTa", PV_G0, PV_G0 + 16)
        for l in range(self.nl):
            self.layer(l, l == self.nl - 1)


def tile_gu(w, nl):
    w = np.asarray(w[:nl], np.float32)
    return np.ascontiguousarray(w.reshape(nl, NEXP, 16, 128, 8, 128).transpose(0, 1, 4, 3, 2, 5)).reshape(nl, NEXP, 8, 128, 16 * 128)


def tile_down(w, nl):
    w = np.asarray(w[:nl], np.float32)
    return np.ascontiguousarray(w.reshape(nl, NEXP, 2, 4, 128, 8, 256).transpose(0, 1, 2, 5, 4, 3, 6)).reshape(nl, NEXP, 2, 8, 128, 4 * 256)


def make_inputs(inp, nl, b):
    C = host_consts()
    m = dict(xT=np.ascontiguousarray(np.asarray(inp['x'][b], np.float32).T), pv=inp['_pv'], w_in=inp['w_in'][:nl],
             rpbT=inp['_rpbT'], hy_f_w1=inp['hy_f_w1'][:nl], hy_f_w2=inp['hy_f_w2'][:nl], hy_f_w3=inp['hy_f_w3'][:nl],
             hy_f_w4=inp['hy_f_w4'][:nl], hy_skip=inp['hy_skip'][:nl], w_attn_br=inp['w_attn_br'][:nl], w_hy_br=inp['w_hy_br'][:nl],
             w_cf_br=inp['w_cf_br'][:nl], w_o=inp['w_o'][:nl], w_router=inp['w_router'], b_router=np.asarray(inp['b_router']).reshape(1, 16),
             moe_w_gate=tile_gu(inp['moe_w_gate'], nl), moe_w_up=tile_gu(inp['moe_w_up'], nl), moe_w_down=tile_down(inp['moe_w_down'], nl),
             cmat=C['cmat'], smat=C['smat'], gmat=C['gmat'], cmatT=C['cmatT'], smatT=C['smatT'], zT=C['zT'], dec=C['dec'],
             mfull=C['mfull'].reshape(128, -1), mint=C['mint'].reshape(128, -1), ident=C['ident'])
    return m


def kernel(**inputs):
    inp = {k: np.asarray(v) for k, v in inputs.items()}
    nl = DEPTH
    inp['_pv'] = pack_pv(inp, nl)
    inp['_rpbT'] = expand_rpb(inp['attn_rpb'][:nl]).reshape(nl, 12, 128, 23 * 64)
    P = Prog(nl)
    P.full()
    shared = make_inputs(inp, nl, 0)
    in_maps = []
    for b in range(8):
        m = dict(shared)
        m['xT'] = np.ascontiguousarray(inp['x'][b].astype(np.float32).T)
        in_maps.append({k: np.ascontiguousarray(v, dtype=np.float32) if v.dtype != np.float32 or not v.flags.c_contiguous else v for k, v in m.items()})
    res = run_bass_kernel_spmd(P.nc, in_maps, core_ids=list(range(8)))
    out = np.stack([np.ascontiguousarray(res.results[b]["outT"].T) for b in range(8)], 0)
    return out.astype(np.float32)
```
